# Optimizing a Trainium2 kernel written in Bass

```python
import jax, jax.numpy as jnp
from jax import lax
import numpy as np

D_MODEL = 1024
BATCH = 4
SEQ = 8192
DEPTH = 2

CHUNK = 64
LRU_WIDTH = D_MODEL // 2
LRU_BLOCKS = 8
LRU_BLOCK_DIM = LRU_WIDTH // LRU_BLOCKS
CONV_WIDTH = 4
LRU_C = 8.0
SB_HEADS = 8
SB_HEAD_DIM = (D_MODEL - LRU_WIDTH) // SB_HEADS
SB_WIDTH = SB_HEADS * SB_HEAD_DIM
MIX_WIDTH = LRU_WIDTH + SB_WIDTH
IN_WIDTH = 2 * LRU_WIDTH + 3 * SB_WIDTH
Q_BLOCK = 128
D_FF = 3 * D_MODEL
N_EXPERTS = 8
TOP_K = 2
EXPERT_BLOCK = 256
N_DENSE = (DEPTH + 1) // 2
N_MOE = DEPTH // 2
NORM_EPS = 1e-6

kernel_name = 'hymba_rglru_stickbreaking_moe_trunk'


def rms_norm(x, g):
    xf = x.astype(jnp.float32)
    y = xf * lax.rsqrt(jnp.mean(xf * xf, axis=-1, keepdims=True) + NORM_EPS)
    return (y * g.astype(jnp.float32)).astype(x.dtype)


def causal_depthwise_conv(x, w, b):
    s = x.shape[1]
    xp = jnp.pad(x, ((0, 0), (CONV_WIDTH - 1, 0), (0, 0)))
    return b + sum(w[j] * xp[:, j:j + s] for j in range(CONV_WIDTH))


def rg_lru(x, w_r, b_r, w_i, b_i, lam):
    bsz, s, _ = x.shape
    xb = x.reshape(bsz, s, LRU_BLOCKS, LRU_BLOCK_DIM)
    r = jax.nn.sigmoid((jnp.einsum('bsgi,gij->bsgj', xb, w_r) + b_r).astype(jnp.float32)).reshape(bsz, s, LRU_WIDTH)
    i = jax.nn.sigmoid((jnp.einsum('bsgi,gij->bsgj', xb, w_i) + b_i).astype(jnp.float32)).reshape(bsz, s, LRU_WIDTH)
    log_a = -LRU_C * r * jax.nn.softplus(-lam.astype(jnp.float32))
    a = jnp.exp(log_a)
    u = jnp.sqrt(-jnp.expm1(2.0 * log_a)) * (i * x.astype(jnp.float32))

    def combine(left, right):
        a_l, u_l = left
        a_r, u_r = right
        return a_l * a_r, a_r * u_l + u_r

    _, h = lax.associative_scan(combine, (a, u), axis=1)
    return h.astype(x.dtype)


def stick_breaking_attention(q, k, v):
    bsz, s, h, dh = q.shape
    n_blocks = s // Q_BLOCK
    scale = dh ** -0.5
    kf = k.astype(jnp.float32)
    vf = v.astype(jnp.float32)
    qb = q.astype(jnp.float32).reshape(bsz, n_blocks, Q_BLOCK, h, dh).transpose(1, 0, 3, 2, 4)
    key_pos = jnp.arange(s)

    def block(args):
        q_blk, blk_idx = args
        z = jnp.einsum('bhqd,bkhd->bhqk', q_blk, kf) * scale
        q_pos = blk_idx * Q_BLOCK + jnp.arange(Q_BLOCK)
        strict = key_pos[None, :] < q_pos[:, None]
        log_1m = jnp.where(strict, jax.nn.log_sigmoid(-z), 0.0)
        rev = lax.cumsum(log_1m, axis=log_1m.ndim - 1, reverse=True)
        later = jnp.concatenate([rev[..., 1:], jnp.zeros_like(rev[..., :1])], axis=-1)
        w = jnp.where(strict, jnp.exp(jax.nn.log_sigmoid(z) + later), 0.0)
        return jnp.einsum('bhqk,bkhd->bqhd', w, vf)

    out = lax.map(block, (qb, jnp.arange(n_blocks)))
    return out.transpose(1, 0, 2, 3, 4).reshape(bsz, s, h * dh).astype(q.dtype)


def hybrid_mixer(h, w_in, conv_w, conv_b, w_r, b_r, w_i, b_i, lam, g_lru, g_sb, w_out):
    bsz, s, _ = h.shape
    proj = h @ w_in
    splits = [LRU_WIDTH, 2 * LRU_WIDTH, 2 * LRU_WIDTH + SB_WIDTH, 2 * LRU_WIDTH + 2 * SB_WIDTH]
    x_lru, gate, q, k, v = jnp.split(proj, splits, axis=-1)
    x_lru = causal_depthwise_conv(x_lru, conv_w, conv_b)
    y_lru = rg_lru(x_lru, w_r, b_r, w_i, b_i, lam) * jax.nn.gelu(gate)
    shp = (bsz, s, SB_HEADS, SB_HEAD_DIM)
    y_sb = stick_breaking_attention(q.reshape(shp), k.reshape(shp), v.reshape(shp))
    y = jnp.concatenate([rms_norm(y_lru, g_lru), rms_norm(y_sb, g_sb)], axis=-1)
    return y @ w_out


def swiglu(h, w_g, w_u, w_d):
    return (jax.nn.silu(h @ w_g) * (h @ w_u)) @ w_d


def moe_swiglu(h, w_router, w_g, w_u, w_d):
    bsz, s, d = h.shape
    hf = h.reshape(-1, d)
    n_tok = hf.shape[0]
    n_assign = n_tok * TOP_K
    logits = (hf @ w_router).astype(jnp.float32)
    top_logit, top_idx = lax.top_k(logits, TOP_K)
    gates = jax.nn.softmax(top_logit, axis=-1)
    flat_e = top_idx.reshape(-1)
    flat_tok = jnp.repeat(jnp.arange(n_tok, dtype=jnp.int32), TOP_K)
    flat_gate = gates.reshape(-1)
    order = jnp.argsort(flat_e)
    e_sorted = flat_e[order]
    counts = jnp.bincount(flat_e, length=N_EXPERTS)
    padded = (counts + EXPERT_BLOCK - 1) // EXPERT_BLOCK * EXPERT_BLOCK
    pad_end = jnp.cumsum(padded)
    pad_start = pad_end - padded
    start = jnp.cumsum(counts) - counts
    dest = pad_start[e_sorted] + jnp.arange(n_assign) - start[e_sorted]
    n_blocks = -(-n_assign // EXPERT_BLOCK) + N_EXPERTS
    n_rows = n_blocks * EXPERT_BLOCK
    row_tok = jnp.zeros((n_rows,), jnp.int32).at[dest].set(flat_tok[order])
    row_gate = jnp.zeros((n_rows,), jnp.float32).at[dest].set(flat_gate[order])
    block_start = jnp.arange(n_blocks) * EXPERT_BLOCK
    block_expert = jnp.minimum(jnp.searchsorted(pad_end, block_start, side='right'), N_EXPERTS - 1)
    x_rows = hf[row_tok].reshape(n_blocks, EXPERT_BLOCK, d)

    def expert_block(args):
        xb, e = args
        return swiglu(xb, w_g[e], w_u[e], w_d[e])

    y_rows = lax.map(expert_block, (x_rows, block_expert)).reshape(n_rows, d)
    y = jnp.zeros_like(hf).at[row_tok].add(y_rows * row_gate[:, None].astype(hf.dtype))
    return y.reshape(bsz, s, d)


def setup_inputs(seed: int = 0) -> dict:
    key = jax.random.key(seed)
    ks = jax.random.split(key, 24)
    f32 = jnp.float32
    nrm = lambda k, shp, sc: jax.random.normal(k, shp, f32) * sc
    a0 = jax.random.uniform(ks[9], (DEPTH, LRU_WIDTH), f32, 0.9, 0.999)
    sig = a0 ** (1.0 / LRU_C)
    lru_lambda = jnp.log(sig) - jnp.log1p(-sig)
    return {
        'x': nrm(ks[0], (BATCH, SEQ, D_MODEL), 1.0),
        'mix_norm': 1.0 + nrm(ks[1], (DEPTH, D_MODEL), 0.01),
        'w_in': nrm(ks[2], (DEPTH, D_MODEL, IN_WIDTH), D_MODEL ** -0.5),
        'conv_w': nrm(ks[3], (DEPTH, CONV_WIDTH, LRU_WIDTH), CONV_WIDTH ** -0.5),
        'conv_b': nrm(ks[4], (DEPTH, LRU_WIDTH), 0.01),
        'w_rgate': nrm(ks[5], (DEPTH, LRU_BLOCKS, LRU_BLOCK_DIM, LRU_BLOCK_DIM), LRU_BLOCK_DIM ** -0.5),
        'b_rgate': nrm(ks[6], (DEPTH, LRU_BLOCKS, LRU_BLOCK_DIM), 0.01),
        'w_igate': nrm(ks[7], (DEPTH, LRU_BLOCKS, LRU_BLOCK_DIM, LRU_BLOCK_DIM), LRU_BLOCK_DIM ** -0.5),
        'b_igate': nrm(ks[8], (DEPTH, LRU_BLOCKS, LRU_BLOCK_DIM), 0.01),
        'lru_lambda': lru_lambda,
        'lru_out_norm': 1.0 + nrm(ks[10], (DEPTH, LRU_WIDTH), 0.01),
        'sb_out_norm': 1.0 + nrm(ks[11], (DEPTH, SB_WIDTH), 0.01),
        'w_out': nrm(ks[12], (DEPTH, MIX_WIDTH, D_MODEL), MIX_WIDTH ** -0.5),
        'ffn_norm': 1.0 + nrm(ks[13], (DEPTH, D_MODEL), 0.01),
        'dense_w_gate': nrm(ks[14], (N_DENSE, D_MODEL, D_FF), D_MODEL ** -0.5),
        'dense_w_up': nrm(ks[15], (N_DENSE, D_MODEL, D_FF), D_MODEL ** -0.5),
        'dense_w_down': nrm(ks[16], (N_DENSE, D_FF, D_MODEL), D_FF ** -0.5),
        'router_w': nrm(ks[17], (N_MOE, D_MODEL, N_EXPERTS), D_MODEL ** -0.5),
        'moe_w_gate': nrm(ks[18], (N_MOE, N_EXPERTS, D_MODEL, D_FF), D_MODEL ** -0.5),
        'moe_w_up': nrm(ks[19], (N_MOE, N_EXPERTS, D_MODEL, D_FF), D_MODEL ** -0.5),
        'moe_w_down': nrm(ks[20], (N_MOE, N_EXPERTS, D_FF, D_MODEL), D_FF ** -0.5),
        'final_norm': 1.0 + nrm(ks[21], (D_MODEL,), 0.01),
    }


def reference(x, mix_norm, w_in, conv_w, conv_b, w_rgate, b_rgate, w_igate, b_igate, lru_lambda,
              lru_out_norm, sb_out_norm, w_out, ffn_norm, dense_w_gate, dense_w_up, dense_w_down,
              router_w, moe_w_gate, moe_w_up, moe_w_down, final_norm):
    for layer in range(DEPTH):
        h = rms_norm(x, mix_norm[layer])
        x = x + hybrid_mixer(h, w_in[layer], conv_w[layer], conv_b[layer], w_rgate[layer], b_rgate[layer],
                             w_igate[layer], b_igate[layer], lru_lambda[layer], lru_out_norm[layer],
                             sb_out_norm[layer], w_out[layer])
        h = rms_norm(x, ffn_norm[layer])
        j = layer // 2
        if layer % 2 == 0:
            x = x + swiglu(h, dense_w_gate[j], dense_w_up[j], dense_w_down[j])
        else:
            x = x + moe_swiglu(h, router_w[j], moe_w_gate[j], moe_w_up[j], moe_w_down[j])
    return rms_norm(x, final_norm)
```

```python
from contextlib import ExitStack
import numpy as np
import concourse.bass as bass
import concourse.mybir as mybir

F32 = mybir.dt.float32
BF16 = mybir.dt.bfloat16
U32 = mybir.dt.uint32
AF = mybir.ActivationFunctionType
ALU = mybir.AluOpType
AX = mybir.AxisListType

COMPUTE = ("pe", "act", "dve", "pool")


class Prog:
    def __init__(self, nc, stack):
        self.nc = nc
        self.stack = stack
        self.q = {k: [] for k in ("pe", "act", "dve", "pool", "sp")}
        self.eng = {"pe": nc.tensor, "act": nc.scalar, "dve": nc.vector,
                    "pool": nc.gpsimd, "sp": nc.sync}
        self.sems = {}
        self.count = {}
        self.waited = {}
        self.last_w = {}
        self.readers = {}
        self.n_ops = 0
        self.n_waits = 0
        for k in COMPUTE:
            self._sem("E_" + k)

    def _sem(self, key):
        if key not in self.sems:
            self.sems[key] = self.stack.enter_context(self.nc.semaphore(key))
            self.count[key] = 0
        return self.sems[key]

    def begin_phase(self, tag):
        self.ph = tag
        self.pstack = ExitStack()
        self.pstack.__enter__()

    def end_phase(self):
        self.drain_all()
        self.run()
        self.q = {k: [] for k in self.q}
        self.last_w = {}
        self.readers = {}
        self.pstack.__exit__(None, None, None)

    def drain_all(self):
        for qname in self.q:
            waits = []
            for k, v in self.count.items():
                if v > 0 and self.waited.get((qname, k), 0) < v:
                    self.waited[(qname, k)] = v
                    waits.append((self.sems[k], v))
            def emit(e, waits=waits):
                for s, v in waits:
                    e.wait_ge(s, v)
            self.q[qname].append(emit)

    def sbuf(self, name, shape, dtype):
        return self.pstack.enter_context(self.nc.sbuf_tensor(f"s{self.ph}_" + name, list(shape), dtype))

    def psum(self, name, shape, dtype=F32):
        return self.pstack.enter_context(self.nc.psum_tensor(f"p{self.ph}_" + name, list(shape), dtype))

    def collective(self, kind, ins, outs, groups, reads, writes, slot):
        key = "C_" + slot
        self._sem(key)
        deps = self._deps(reads, writes, skip_key=None)
        waits = self._emit_waits("pool", deps)
        self.count[key] += 1
        val = self.count[key]
        sem = self.sems[key]

        def emit(e, waits=waits, sem=sem):
            for s, v in waits:
                e.wait_ge(s, v)
            e.collective_compute(kind, ALU.bypass, replica_groups=groups, ins=ins, outs=outs).then_inc(sem, 1)
        self.q["pool"].append(emit)
        self._commit(reads, writes, (key, val))

    def _deps(self, reads, writes, skip_key=None):
        deps = {}
        def add(ev):
            if ev is None:
                return
            k, v = ev
            if k == skip_key:
                return
            if deps.get(k, 0) < v:
                deps[k] = v
        for r in reads:
            add(self.last_w.get(r))
        for w in writes:
            add(self.last_w.get(w))
            for ev in self.readers.get(w, ()):
                add(ev)
        return deps

    def _commit(self, reads, writes, ev):
        for r in reads:
            self.readers.setdefault(r, []).append(ev)
        for w in writes:
            self.last_w[w] = ev
            self.readers[w] = []

    def _emit_waits(self, qname, deps):
        out = []
        for k, v in deps.items():
            if self.waited.get((qname, k), 0) >= v:
                continue
            self.waited[(qname, k)] = v
            out.append((self.sems[k], v))
        return out

    def op(self, qname, fn, reads=(), writes=(), pe_acc=False):
        key = "E_" + qname
        deps = self._deps(reads, writes, skip_key=(key if (qname == "pe") else None))
        waits = self._emit_waits(qname, deps)
        self.count[key] += 1
        val = self.count[key]
        sem = self.sems[key]
        self.n_ops += 1
        self.n_waits += len(waits)

        def emit(e, fn=fn, waits=waits, sem=sem):
            for s, v in waits:
                e.wait_ge(s, v)
            fn(e).then_inc(sem, 1)
        self.q[qname].append(emit)
        self._commit(reads, writes, (key, val))

    def I(self, qname, method, *args, reads=(), writes=(), **kw):
        self.op(qname, (lambda e, m=method, a=args, k=kw: getattr(e, m)(*a, **k)), reads, writes)

    def dma(self, qname, out, in_, reads=(), writes=(), slot=None, **kw):
        assert slot is not None
        key = "D_" + slot
        self._sem(key)
        deps = self._deps(reads, writes, skip_key=key)
        waits = self._emit_waits(qname, deps)
        self.count[key] += 16
        val = self.count[key]
        sem = self.sems[key]
        self.n_ops += 1
        self.n_waits += len(waits)

        def emit(e, waits=waits, sem=sem, out=out, in_=in_, kw=kw):
            for s, v in waits:
                e.wait_ge(s, v)
            e.dma_start(out=out, in_=in_, **kw).then_inc(sem, 16)
        self.q[qname].append(emit)
        self._commit(reads, writes, (key, val))

    def finish(self, qname, resources):
        deps = self._deps(resources, resources)
        waits = self._emit_waits(qname, deps)

        def emit(e, waits=waits):
            for s, v in waits:
                e.wait_ge(s, v)
        self.q[qname].append(emit)

    def run(self):
        nc = self.nc
        with nc.Block() as block:
            @block.tensor
            def _(e):
                for f in self.q["pe"]:
                    f(e)

            @block.scalar
            def _(e):
                for f in self.q["act"]:
                    f(e)

            @block.vector
            def _(e):
                for f in self.q["dve"]:
                    f(e)

            @block.gpsimd
            def _(e):
                for f in self.q["pool"]:
                    f(e)

            @block.sync
            def _(e):
                for f in self.q["sp"]:
                    f(e)


NEG = -30000.0
EPS = 1e-6
TC = 512


def emit_p1(P, S, d, L, x_chunk, ysrc, piece_done=None, chunks_per_piece=2):
    nc = P.nc
    NT = S // TC
    NKT = S // 128
    d = {"gk": d["gk1"][L], "w_in": d["w_in"][L], "conv_w": d["conv_w"][L], "vecs": d["vecs1"][L],
         "w_r": d["w_r"][L], "w_i": d["w_i"][L], "consts": d["consts1"]}
    if True:
        if True:
            pass
        cst = P.sbuf("cst", [128, 3 * 128 + 4 * 512], BF16)
        P.dma("pool", cst[:], d["consts"], writes=["cst"], slot="cst")
        ones = cst[:, 0:128]
        trineg = cst[:, 128:256]
        ident = cst[:, 256:384]
        masks = [cst[:, 384 + i * 512: 384 + (i + 1) * 512] for i in range(4)]
        negones = P.sbuf("negones", [128, 128], BF16)
        P.op("dve", lambda e: e.tensor_scalar(negones[:], ones, -1.0, None, ALU.mult), reads=["cst"], writes=["negones"])

        gk = P.sbuf("gk", [128, 8], F32)
        P.dma("sp", gk[:], d["gk"], writes=["gk"], slot="gk")
        convw = P.sbuf("convw", [128, 2, 4], F32)
        P.dma("sp", convw[:], d["conv_w"], writes=["convw"], slot="convw")
        vecs = P.sbuf("vecs", [128, 2, 4], F32)
        P.dma("sp", vecs[:], d["vecs"], writes=["vecs"], slot="vecs")
        c1 = P.sbuf("c1", [128, 2], F32)
        for cc in range(2):
            P.op("act", lambda e, cc=cc: e.activation(c1[:, cc:cc + 1], vecs[:, cc, 3:4], AF.Exp, scale=-1.0), reads=["vecs"], writes=["c1"])
        P.op("act", lambda e: e.activation(c1[:], c1[:], AF.Ln, bias=1.0), reads=["c1"], writes=["c1"])
        P.op("dve", lambda e: e.tensor_scalar(c1[:], c1[:], -8.0, None, ALU.mult), reads=["c1"], writes=["c1"])

        bd_f = P.sbuf("bd_f", [128, 4, 128], F32)
        P.op("pool", lambda e: e.memset(bd_f[:], 0.0), writes=["bd_f"])
        for gi, nm in enumerate(("w_r", "w_i")):
            for cc in range(2):
                for hb in range(2):
                    P.dma("sp", bd_f[hb * 64:(hb + 1) * 64, gi * 2 + cc, hb * 64:(hb + 1) * 64], d[nm][cc * 2 + hb],
                          reads=[], writes=["bd_f"], slot="bd_f")
        bd = P.sbuf("bd", [128, 4, 128], BF16)
        P.op("dve", lambda e: e.tensor_copy(bd[:], bd_f[:]), reads=["bd_f"], writes=["bd"])

        wbf = P.sbuf("wbf", [128, 8, 1280], BF16)
        wst = [P.sbuf(f"wst{i}", [128, 1280], F32) for i in range(2)]
        w_in_v = d["w_in"].rearrange("(k p) c -> p k c", p=128)
        for k in range(8):
            b = k % 2
            P.dma("sp", wst[b][:], w_in_v[:, k, :], writes=[f"wst{b}"], slot=f"wst{b}")
            P.op("dve", lambda e, k=k, b=b: e.tensor_scalar(wbf[:, k, :], wst[b][:], gk[:, k:k + 1], None, ALU.mult),
                 reads=[f"wst{b}", "gk"], writes=["wbf"])
        P.op("dve", lambda e: e.tensor_scalar(wbf[:, :, 512:768], wbf[:, :, 512:768], 0.125, None, ALU.mult), reads=["wbf"], writes=["wbf"])

        kT = P.sbuf("kT", [128, 2, S], BF16)
        V = P.sbuf("V", [128, NKT, 256], BF16)
        xt = P.sbuf("xt", [128, 8, TC], F32)
        sq = P.sbuf("sq", [128, 8, TC], BF16)
        hT = P.sbuf("hT", [128, 8, TC], BF16)
        rstd = P.sbuf("rstd", [128, TC], F32)
        xl = P.sbuf("xl", [128, 2, 3 + TC], F32)
        P.op("pool", lambda e: e.memset(xl[:], 0.0), writes=["xl0", "xl1"])
        xc_ = [P.sbuf(f"xc{i}", [128, TC], F32) for i in range(2)]
        xcb_ = [P.sbuf(f"xcb{i}", [128, TC], BF16) for i in range(2)]
        rr_ = [P.sbuf(f"rr{i}", [128, TC], F32) for i in range(2)]
        ii_ = [P.sbuf(f"ii{i}", [128, TC], F32) for i in range(2)]
        ss_ = [P.sbuf(f"ss{i}", [128, TC], F32) for i in range(2)]
        hb_ = [P.sbuf(f"hbuf{i}", [128, 2, TC], F32) for i in range(2)]
        gt_ = [P.sbuf(f"gt{i}", [128, TC], F32) for i in range(2)]
        t1_ = [P.sbuf(f"t1{i}", [128, TC], F32) for i in range(2)]
        yl = P.sbuf("yl", [128, 2, TC], F32)
        ebuf = [P.sbuf(f"e{i}", [128, 2 * TC], F32) for i in range(2)]
        spb = [P.sbuf(f"sp{i}", [128, 2 * TC], BF16) for i in range(2)]
        wb = [P.sbuf(f"w{i}", [128, 2 * TC], BF16) for i in range(2)]
        Rb = [P.sbuf(f"R{i}", [128, 2 * TC], BF16) for i in range(2)]
        ost = [P.sbuf(f"ost{i}", [128, TC], F32) for i in range(2)]
        qT2 = P.sbuf("qT2", [128, 4, TC], BF16)
        nvecs = P.sbuf("nvecs", [128, 2, 4], F32)
        P.I("dve", "tensor_scalar", nvecs[:], vecs[:], -1.0, None, ALU.mult, reads=["vecs"], writes=["nvecs"])
        psA2 = [P.psum(f"psA{i}", [128, 2 * TC]) for i in range(3)]
        psO1 = P.psum("psO", [128, TC])
        pq_ = P.psum("pq", [128, TC])
        pq = [pq_, pq_]
        npp = [0]

        def prep(tc):
            t0 = tc * TC
            qb = (tc % 2) * 2
            P.dma("sp", xt[:], x_chunk(tc), writes=["xt"], slot="xt")
            P.I("act", "activation", sq[:], xt[:], AF.Square, reads=["xt"], writes=["sq"])
            yield
            for k in range(8):
                P.I("pe", "matmul", pq[0][:], ones, sq[:, k, :], start=(k == 0), stop=(k == 7), reads=["cst", "sq"], writes=["pq0"])
                if k % 4 == 3:
                    yield
            P.I("act", "activation", rstd[:], pq[0][:], AF.Ln, scale=1.0 / 1024, bias=EPS, reads=["pq0"], writes=["rstd"])
            P.I("act", "activation", rstd[:], rstd[:], AF.Exp, scale=-0.5, reads=["rstd"], writes=["rstd"])
            yield
            for k in range(8):
                en = "dve" if k % 2 == 0 else "pool"
                P.I(en, "tensor_tensor", hT[:, k, :], xt[:, k, :], rstd[:], ALU.mult, reads=["xt", "rstd"], writes=[f"hT{k}"])
                if k % 4 == 3:
                    yield
            for cc in range(8):
                pb = npp[0] % 2
                npp[0] += 1
                ps, psn = pq[pb], "pq0"
                for k in range(8):
                    P.I("pe", "matmul", ps[:], wbf[:, k, cc * 128:(cc + 1) * 128], hT[:, k, :], start=(k == 0), stop=(k == 7),
                        reads=["wbf", f"hT{k}"], writes=[psn])
                    if k % 4 == 3:
                        yield
                if cc < 2:
                    P.I("dve", "tensor_copy", xl[:, cc, 3:3 + TC], ps[:], reads=[psn], writes=[f"xl{cc}"])
                elif cc < 4:
                    P.I("dve", "tensor_copy", gt_[cc - 2][:], ps[:], reads=[psn], writes=[f"gt{cc - 2}"])
                    if cc == 3:
                        yield
                        chains = [lru(0, tc, t0), lru(1, tc, t0)]
                        while chains:
                            for g in list(chains):
                                if next(g, "done") == "done":
                                    chains.remove(g)
                                else:
                                    yield
                elif cc < 6:
                    P.I("dve", "tensor_copy", qT2[:, qb + cc - 4, :], ps[:], reads=[psn], writes=[f"qT{qb + cc - 4}"])
                else:
                    P.I("dve", "tensor_copy", kT[:, cc - 6, t0:t0 + TC], ps[:], reads=[psn], writes=[f"kT{cc - 6}_{tc}"])
                yield
            for j in range(4):
                kt = tc * 4 + j
                for k in range(8):
                    P.I("pe", "matmul", pq[1][:, 0:256], hT[:, k, j * 128:(j + 1) * 128], wbf[:, k, 1024:1280], start=(k == 0), stop=(k == 7),
                        reads=["wbf", f"hT{k}"], writes=["pq0"])
                    if k % 4 == 3:
                        yield
                P.I("dve", "tensor_copy", V[:, kt, :], pq[1][:, 0:256], reads=["pq0"], writes=[f"V{kt}"])
                yield

        def sigmoid_chain(buf, bn, src_ap, src_names, scale, bias):
            P.I("act", "activation", buf, src_ap, AF.Exp, scale=-scale, bias=bias, reads=src_names, writes=[bn])
            P.I("act", "activation", buf, buf, AF.Ln, bias=1.0, reads=[bn], writes=[bn])
            P.I("act", "activation", buf, buf, AF.Exp, scale=-1.0, reads=[bn], writes=[bn])

        def lru(cc, tc, t0):
            X = f"xl{cc}"
            xc, xcb, rr, ii, ss, gt, t1 = xc_[cc], xcb_[cc], rr_[cc], ii_[cc], ss_[cc], gt_[cc], t1_[cc]
            XC, XCB, RR, II, SS, GT, T1 = f"xc{cc}", f"xcb{cc}", f"rr{cc}", f"ii{cc}", f"ss{cc}", f"gt{cc}", f"t1{cc}"
            P.I("dve", "tensor_scalar", xc[:], xl[:, cc, 3:3 + TC], convw[:, cc, 3:4], vecs[:, cc, 0:1], ALU.mult, ALU.add,
                reads=[X, "convw", "vecs"], writes=[XC])
            yield
            for j in range(3):
                P.I("dve", "scalar_tensor_tensor", xc[:], xl[:, cc, j:j + TC], convw[:, cc, j:j + 1], xc[:], ALU.mult, ALU.add,
                    reads=[X, "convw", XC], writes=[XC])
                yield
            P.I("pool", "tensor_copy", xl[:, cc, 0:3], xl[:, cc, TC:TC + 3], reads=[X], writes=[X])
            P.I("pool", "tensor_copy", xcb[:], xc[:], reads=[XC], writes=[XCB])
            yield
            P.I("pe", "matmul", pq[0][:], bd[:, cc, :], xcb[:], start=True, stop=True, reads=["bd", XCB], writes=["pq0"])
            sigmoid_chain(rr[:], RR, pq[0][:], ["pq0", "nvecs"], 1.0, nvecs[:, cc, 1:2])
            yield
            P.I("pe", "matmul", pq[0][:], bd[:, 2 + cc, :], xcb[:], start=True, stop=True, reads=["bd", XCB], writes=["pq0"])
            sigmoid_chain(ii[:], II, pq[0][:], ["pq0", "nvecs"], 1.0, nvecs[:, cc, 2:3])
            yield
            P.I("act", "activation", rr[:], rr[:], AF.Exp, scale=c1[:, cc:cc + 1], reads=[RR, "c1"], writes=[RR])
            P.I("dve", "scalar_tensor_tensor", ss[:], rr[:], 0.9999999, rr[:], ALU.min, ALU.mult, reads=[RR], writes=[SS])
            yield
            P.I("act", "activation", ss[:], ss[:], AF.Ln, scale=-1.0, bias=1.0, reads=[SS], writes=[SS])
            P.I("act", "activation", ss[:], ss[:], AF.Exp, scale=0.5, reads=[SS], writes=[SS])
            P.I("dve", "tensor_tensor", ii[:], ii[:], xc[:], ALU.mult, reads=[II, XC], writes=[II])
            yield
            P.I("dve", "tensor_tensor", ii[:], ii[:], ss[:], ALU.mult, reads=[II, SS], writes=[II])
            hcur, hprev = hb_[tc % 2], hb_[(tc + 1) % 2]
            Hc, Hp = f"h{tc % 2}_{cc}", f"h{(tc + 1) % 2}_{cc}"
            yield
            if tc == 0:
                P.I("dve", "tensor_tensor_scan", hcur[:, cc, :], rr[:], ii[:], 0.0, ALU.mult, ALU.add, reads=[RR, II], writes=[Hc])
            else:
                P.I("dve", "tensor_tensor_scan", hcur[:, cc, :], rr[:], ii[:], hprev[:, cc, TC - 1:TC], ALU.mult, ALU.add,
                    reads=[RR, II, Hp], writes=[Hc])
            yield
            P.I("pool", "tensor_tensor", t1[:], gt[:], gt[:], ALU.mult, reads=[GT], writes=[T1])
            P.I("dve", "tensor_scalar", t1[:], t1[:], 0.044715, 1.0, ALU.mult, ALU.add, reads=[T1], writes=[T1])
            yield
            P.I("pool", "tensor_tensor", t1[:], t1[:], gt[:], ALU.mult, reads=[T1, GT], writes=[T1])
            sigmoid_chain(t1[:], T1, t1[:], [T1], 1.5957691216057308, 0.0)
            yield
            P.I("pool", "tensor_tensor", t1[:], t1[:], gt[:], ALU.mult, reads=[T1, GT], writes=[T1])
            P.I("dve", "tensor_tensor", yl[:, cc, :], hcur[:, cc, :], t1[:], ALU.mult, reads=[Hc, T1], writes=[f"yl{cc}"])
            P.dma("sp", ysrc(cc * 128, (cc + 1) * 128, t0, TC), yl[:, cc, :], reads=[f"yl{cc}"], writes=[f"ysrc{tc // chunks_per_piece}_yl{cc}"], slot=f"yl{cc}")
            yield

        def attention(tc, nxt, n_units):
            t0 = tc * TC
            qb = (tc % 2) * 2
            nk = tc * 4 + 4
            steps_left = 2 * (nk + 1)
            units_left = n_units if nxt is not None else 0
            An = lambda s: [f"A{s}h0", f"A{s}h1"]
            for hpair in range(2):
                hc = hpair

                def qk(n):
                    kt = nk - 1 - n
                    s = n % 3
                    diag = kt >= tc * 4
                    for hh in range(2):
                        hp = hh * 64
                        P.I("pe", "matmul", psA2[s][:, hh * TC:(hh + 1) * TC], kT[hp:hp + 64, hc, kt * 128:(kt + 1) * 128], qT2[hp:hp + 64, qb + hc, :],
                            start=True, stop=(not diag), reads=[f"kT{hc}_{kt // 4}", f"qT{qb + hc}"], writes=[f"A{s}h{hh}"])
                    if diag:
                        for hh in range(2):
                            P.I("pe", "matmul", psA2[s][:, hh * TC:(hh + 1) * TC], ident, masks[kt - tc * 4], start=False, stop=True,
                                reads=["cst"], writes=[f"A{s}h{hh}"])

                def cum(n):
                    s, b = n % 3, n % 2
                    for hh in range(2):
                        P.I("pe", "matmul", psA2[s][:, hh * TC:(hh + 1) * TC], trineg, spb[b][:, hh * TC:(hh + 1) * TC], start=False, stop=(n == 0),
                            skip_group_check=True, reads=["cst", f"sp{b}"], writes=[f"A{s}h{hh}"])
                    if n > 0:
                        for hh in range(2):
                            P.I("pe", "matmul", psA2[s][:, hh * TC:(hh + 1) * TC], negones[:], Rb[1 - b][:, hh * TC:(hh + 1) * TC], start=False, stop=True,
                                skip_group_check=True, reads=["negones", f"R{1 - b}"], writes=[f"A{s}h{hh}"])
                        P.I("dve", "tensor_tensor", Rb[b][:], Rb[1 - b][:], spb[b][:], ALU.add, reads=[f"R{1 - b}", f"sp{b}"], writes=[f"R{b}"])
                    else:
                        P.I("dve", "tensor_copy", Rb[b][:], spb[b][:], reads=[f"sp{b}"], writes=[f"R{b}"])

                def wv(n):
                    kt = nk - 1 - n
                    b = n % 2
                    for hh in range(2):
                        h = 2 * hpair + hh
                        P.I("pe", "matmul", psO1[hh * 64:(hh + 1) * 64, :], V[:, kt, h * 64:(h + 1) * 64], wb[b][:, hh * TC:(hh + 1) * TC],
                            start=(n == 0), stop=(n == nk - 1), reads=[f"V{kt}", f"w{b}"], writes=[f"O{hh}"])

                qk(0)
                qk(1)
                for n in range(nk + 1):
                    if n < nk:
                        s, b = n % 3, n % 2
                        P.I("act", "activation", ebuf[b][:], psA2[s][:], AF.Exp, reads=An(s), writes=[f"e{b}"])
                        P.I("act", "activation", spb[b][:], ebuf[b][:], AF.Ln, bias=1.0, reads=[f"e{b}"], writes=[f"sp{b}"])
                        cum(n)
                    if n >= 1:
                        s1, b1 = (n - 1) % 3, (n - 1) % 2
                        P.I("act", "activation", wb[b1][:], psA2[s1][:], AF.Exp, reads=An(s1), writes=[f"w{b1}"])
                        wv(n - 1)
                    if n + 2 < nk:
                        qk(n + 2)
                    if nxt is not None and units_left > 0:
                        take = -(-units_left // steps_left)
                        for _ in range(take):
                            if next(nxt, "done") == "done":
                                units_left = 0
                                break
                            units_left -= 1
                    steps_left -= 1
                os_ = ost[hpair]
                P.I("dve", "tensor_copy", os_[:], psO1[:], reads=["O0", "O1"], writes=[f"ost{hpair}"])
                P.dma("sp", ysrc(256 + hpair * 128, 256 + (hpair + 1) * 128, t0, TC), os_[:], reads=[f"ost{hpair}"], writes=[f"ysrc{tc // chunks_per_piece}_ost{hpair}"], slot=f"ost{hpair}")
            if nxt is not None:
                for _ in nxt:
                    pass

        n_units = sum(1 for _ in prep(0))
        for tc in range(NT):
            attention(tc, prep(tc + 1) if tc + 1 < NT else None, n_units)
            if piece_done is not None and tc % chunks_per_piece == chunks_per_piece - 1:
                j = tc // chunks_per_piece
                piece_done(j, [f"ysrc{j}_yl0", f"ysrc{j}_yl1", f"ysrc{j}_ost0", f"ysrc{j}_ost1"])
        print("P1 ops", P.n_ops, "waits", P.n_waits)


def p1_consts():
    c = np.zeros((128, 3 * 128 + 4 * 512), np.float32)
    c[:, 0:128] = 1.0
    j = np.arange(128)[:, None]
    s = np.arange(128)[None, :]
    c[:, 128:256] = np.where(j >= s, -1.0, 0.0)
    c[:, 256:384] = np.eye(128)
    t = np.arange(512)[None, :]
    for ki in range(4):
        c[:, 384 + ki * 512: 384 + (ki + 1) * 512] = np.where((ki * 128 + j) < t, 0.0, NEG)
    return c


def p1_inputs_T(xTb, layer, hs, W):
    sl = slice(hs * 256, (hs + 1) * 256)
    w_in = W["w_in"][layer]
    cols = np.concatenate([w_in[:, 0:512][:, sl], w_in[:, 512:1024][:, sl], w_in[:, 1024:1536][:, sl],
                           w_in[:, 1536:2048][:, sl], w_in[:, 2048:2560][:, sl]], axis=1)
    def pc(v):
        return np.ascontiguousarray(v[sl].reshape(2, 128).T)
    conv_w = np.ascontiguousarray(W["conv_w"][layer][:, sl].reshape(4, 2, 128).transpose(2, 1, 0))
    vecs = np.stack([pc(W["conv_b"][layer]), pc(W["b_rgate"][layer].reshape(-1)), pc(W["b_igate"][layer].reshape(-1)),
                     pc(W["lru_lambda"][layer])], axis=2)
    return {
        "gk": np.ascontiguousarray(W["mix_norm"][layer].reshape(8, 128).T),
        "w_in": np.ascontiguousarray(cols),
        "conv_w": conv_w,
        "vecs": np.ascontiguousarray(vecs),
        "w_r": np.ascontiguousarray(W["w_rgate"][layer][hs * 4:(hs + 1) * 4]),
        "w_i": np.ascontiguousarray(W["w_igate"][layer][hs * 4:(hs + 1) * 4]),
        "consts": p1_consts(),
    }


CH = 512


def emit_p2(P, T, TB, moe, final, d, L, x_chunk, y_chunks, out_chunk, out_done=None):
    nc = P.nc
    NB = T // TB
    NCH = TB // CH
    NE = 8 if moe else 1
    j = L // 2
    d = {"vec": d["vec2"][L], "w_out": d["w_out"][L], "consts": d["consts2"], "hmask": d["hmask"],
         "wg": d["moe_wg"] if moe else d["dense_wg"], "wu": d["moe_wu"] if moe else d["dense_wu"],
         "wd": d["moe_wd"] if moe else d["dense_wd"], "w_router": d["w_router"]}
    LRU_K = (0, 1, 4, 5)
    SB_K = (2, 3, 6, 7)
    if True:
        if True:
            pass
        cstf = P.sbuf("cstf", [128, 256 + 1024], F32)
        P.dma("sp", cstf[:], d["consts"], writes=["cstf"], slot="cstf")
        cstb = P.sbuf("cstb", [128, 128], BF16)
        P.I("dve", "tensor_copy", cstb[:], cstf[:, 0:128], reads=["cstf"], writes=["cstb"])
        ones = cstb[:, 0:128]
        onesf = cstf[:, 0:128]
        identf = cstf[:, 128:256]
        sel = [cstf[0:8, 256 + e * 128: 256 + (e + 1) * 128] for e in range(8)]
        vec = P.sbuf("vec", [128, 3, 8], F32)
        P.dma("sp", vec[:], d["vec"], writes=["vec"], slot="vec")

        hmask = P.sbuf("hmask", [128, 2], F32)
        P.dma("sp", hmask[:], d["hmask"], writes=["hmask"], slot="vec")
        xres = P.sbuf("xres", [128, 8, TB], F32)
        hT = P.sbuf("hT", [128, 8, TB], BF16)
        arena = P.sbuf("arena", [128, 24576], BF16)
        def wview(b, which):
            base = b * 12288
            if which == "g":
                return arena[:, base:base + 4096].rearrange("p (k f) -> p k f", k=8)
            if which == "u":
                return arena[:, base + 4096:base + 8192].rearrange("p (k f) -> p k f", k=8)
            return arena[:, base + 8192:base + 12288].rearrange("p (j d) -> p j d", j=4)
        wout = arena[:, 0:8192].rearrange("p (k c) -> p k c", k=8)
        yst = P.sbuf("yst", [128, 8, CH], F32)
        yst2 = arena[:, 12288:12288 + 8192].bitcast(F32).rearrange("p (k t) -> p k t", k=8)
        sq = P.sbuf("sq", [128, 8, CH], BF16)
        rs = [P.sbuf(f"rs{i}", [128, CH], F32) for i in range(2)]
        sb_ = [P.sbuf(f"s{i}", [128, CH], BF16) for i in range(2)]
        sg_ = [P.sbuf(f"sg{i}", [128, CH], BF16) for i in range(2)]
        act = [P.sbuf(f"act{i}", [128, 4, CH], BF16) for i in range(2)]
        psG = [P.psum(f"psG{i}", [128, CH]) for i in range(2)]
        psU = [P.psum(f"psU{i}", [128, CH]) for i in range(2)]
        psD = [P.psum(f"psD{i}", [128, CH]) for i in range(4)]
        psS = [psD[2], psD[3]]
        if moe:
            wr = P.sbuf("wr", [128, 8, 8], F32)
            P.dma("sp", wr[:], d["w_router"].rearrange("(k p) e -> p k e", p=128), writes=["wr"], slot="wr")
            for k in range(8):
                P.I("dve", "tensor_scalar", wr[:, k, :], wr[:, k, :], vec[:, 1, k:k + 1], None, ALU.mult, reads=["wr", "vec"], writes=["wr"])
            GT = P.sbuf("GT", [8, TB], F32)
            Gb = P.sbuf("Gb", [128, TB], BF16)
            lg = P.sbuf("lg", [128, 8], F32)
            mx = P.sbuf("mx", [128, 8], F32)
            gsm = P.sbuf("gsm", [128, 4], F32)
            m1 = P.sbuf("m1", [128, 8], F32)
            m2 = P.sbuf("m2", [128, 8], F32)
            rtok = P.sbuf("rtok", [128, 2], F32)

        cnt = {"g": 0, "d": 0}
        wcnt = [0]

        def rms_bcast(src_k, scale_n, ps, out_rs, kr, src_names):
            for k in kr:
                P.I("act", "activation", sq[:, k, :], src_k(k), AF.Square, reads=src_names(k), writes=[f"sq{k}"])
            for n, k in enumerate(kr):
                P.I("pe", "matmul", ps[0][:], ones, sq[:, k, :], start=(n == 0), stop=(n == len(kr) - 1),
                    reads=["cstb", f"sq{k}"], writes=[ps[1]])
            P.I("act", "activation", out_rs[0][:], ps[0][:], AF.Ln, scale=1.0 / scale_n, bias=EPS, reads=[ps[1]], writes=[out_rs[1]])
            P.I("act", "activation", out_rs[0][:], out_rs[0][:], AF.Exp, scale=-0.5, reads=[out_rs[1]], writes=[out_rs[1]])

        for blk in range(NB):
            b0 = blk * TB
            P.dma("pool", wout, d["w_out"].rearrange("(k p) c -> p k c", p=128), writes=["arena0"], slot="arena0")
            for c in range(NCH):
                P.dma("sp", xres[:, :, c * CH:(c + 1) * CH], x_chunk(blk, c), writes=[f"xres{k}_{c}" for k in range(8)], slot=f"xres{c}")
            for c in range(NCH):
                c0 = c * CH
                yA, yB = y_chunks(blk, c)
                P.dma("sp", yst[:], yA, writes=[f"yst{k}" for k in range(8)], slot="yst")
                P.dma("sp", yst2, yB, writes=["arena1"], slot="yst2")
                for k in range(8):
                    P.I("dve", "tensor_scalar", yst[:, k, :], yst[:, k, :], hmask[:, 0:1], None, ALU.mult, reads=[f"yst{k}", "hmask"], writes=[f"yst{k}"])
                    P.I("dve", "scalar_tensor_tensor", yst[:, k, :], yst2[:, k, :], hmask[:, 1:2], yst[:, k, :], ALU.mult, ALU.add,
                        reads=["arena1", f"yst{k}", "hmask"], writes=[f"yst{k}"])
                for g in range(2):
                    kr = list(LRU_K if g == 0 else SB_K)
                    rms_bcast(lambda k: yst[:, k, :], 512.0, (psS[g], f"psD{2 + g}"), (rs[g], f"rs{g}"), kr, lambda k: [f"yst{k}"])
                    for k in kr:
                        P.I("dve", "scalar_tensor_tensor", hT[:, k, c0:c0 + CH], yst[:, k, :], vec[:, 0, k:k + 1], rs[g][:], ALU.mult, ALU.mult,
                            reads=[f"yst{k}", "vec", f"rs{g}"], writes=[f"hT{k}_{c}"])
                for dc in range(8):
                    pb = cnt["d"] % 4
                    cnt["d"] += 1
                    for k in range(8):
                        P.I("pe", "matmul", psD[pb][:], wout[:, k, dc * 128:(dc + 1) * 128], hT[:, k, c0:c0 + CH], start=(k == 0), stop=(k == 7),
                            reads=["arena0", f"hT{k}_{c}"], writes=[f"psD{pb}"])
                    P.I("dve", "tensor_tensor", xres[:, dc, c0:c0 + CH], xres[:, dc, c0:c0 + CH], psD[pb][:], ALU.add,
                        reads=[f"xres{dc}_{c}", f"psD{pb}"], writes=[f"xres{dc}_{c}"])
            for c in range(NCH):
                c0 = c * CH
                g = c % 2
                rms_bcast(lambda k: xres[:, k, c0:c0 + CH], 1024.0, (psS[g], f"psD{2 + g}"), (rs[g], f"rs{g}"), list(range(8)),
                          lambda k: [f"xres{k}_{c}"])
                for k in range(8):
                    P.I("dve", "scalar_tensor_tensor", hT[:, k, c0:c0 + CH], xres[:, k, c0:c0 + CH], vec[:, 1, k:k + 1], rs[g][:], ALU.mult, ALU.mult,
                        reads=[f"xres{k}_{c}", "vec", f"rs{g}"], writes=[f"hT{k}_{c}"])
                if moe:
                    for j in range(4):
                        tt = c0 + j * 128
                        pg = psG[j % 2]
                        pgn = f"psG{j % 2}"
                        for k in range(8):
                            P.I("pe", "matmul", pg[:, 0:8], xres[:, k, tt:tt + 128], wr[:, k, :], start=(k == 0), stop=(k == 7),
                                reads=[f"xres{k}_{c}", "wr"], writes=[pgn])
                        for k in range(8):
                            P.I("pe", "matmul", pg[:, 8:9], sq[:, k, j * 128:(j + 1) * 128], cstb[:, 0:1], start=(k == 0), stop=(k == 7),
                                reads=[f"sq{k}", "cstb"], writes=[pgn], skip_group_check=True)
                        P.I("act", "activation", rtok[:, 0:1], pg[:, 8:9], AF.Sqrt, scale=1.0 / 1024, bias=EPS, reads=[pgn], writes=["rtok"])
                        P.I("dve", "reciprocal", rtok[:, 1:2], rtok[:, 0:1], reads=["rtok"], writes=["rtok"])
                        P.I("dve", "tensor_scalar", lg[:], pg[:, 0:8], rtok[:, 1:2], None, ALU.mult, reads=[pgn, "rtok"], writes=["lg"])
                        P.I("dve", "max", mx[:], lg[:], reads=["lg"], writes=["mx"])
                        P.I("dve", "tensor_tensor", gsm[:, 0:1], mx[:, 0:1], mx[:, 1:2], ALU.subtract, reads=["mx"], writes=["gsm"])
                        P.I("act", "activation", gsm[:, 1:2], gsm[:, 0:1], AF.Sigmoid, reads=["gsm"], writes=["gsm"])
                        P.I("dve", "tensor_scalar", gsm[:, 2:3], gsm[:, 1:2], -1.0, 1.0, ALU.mult, ALU.add, reads=["gsm"], writes=["gsm"])
                        P.I("dve", "tensor_scalar", m1[:], lg[:], mx[:, 0:1], gsm[:, 1:2], ALU.is_equal, ALU.mult, reads=["lg", "mx", "gsm"], writes=["m1"])
                        P.I("dve", "tensor_scalar", m2[:], lg[:], mx[:, 1:2], gsm[:, 2:3], ALU.is_equal, ALU.mult, reads=["lg", "mx", "gsm"], writes=["m2"])
                        P.I("dve", "tensor_tensor", m1[:], m1[:], m2[:], ALU.add, reads=["m1", "m2"], writes=["m1"])
                        pu = psU[j % 2]
                        pun = f"psU{j % 2}"
                        P.I("pe", "transpose", pu[0:8, 0:128], m1[:], identf, reads=["m1", "cstf"], writes=[pun])
                        P.I("dve", "tensor_copy", GT[:, tt:tt + 128], pu[0:8, 0:128], reads=[pun], writes=[f"GT{c}"])
            for e in range(NE):
                if moe:
                    for c in range(NCH):
                        c0 = c * CH
                        pb = cnt["d"] % 4
                        cnt["d"] += 1
                        P.I("pe", "matmul", psD[pb][:], sel[e], GT[:, c0:c0 + CH], start=True, stop=True, reads=["cstf", f"GT{c}"], writes=[f"psD{pb}"])
                        P.I("act", "activation", Gb[:, c0:c0 + CH], psD[pb][:], AF.Copy, reads=[f"psD{pb}"], writes=[f"Gb{c}"])
                for fg in range(6):
                    wb = wcnt[0] % 2
                    wcnt[0] += 1
                    wg_, wu_, wd_ = wview(wb, "g"), wview(wb, "u"), wview(wb, "d")
                    an = f"arena{wb}"
                    P.dma("pool", wg_, d["wg"][e].rearrange("(k p) f -> p k f", p=128)[:, :, fg * 512:(fg + 1) * 512], writes=[an], slot=an)
                    P.dma("pool", wu_, d["wu"][e].rearrange("(k p) f -> p k f", p=128)[:, :, fg * 512:(fg + 1) * 512], writes=[an], slot=an)
                    P.dma("pool", wd_, d["wd"][e][fg * 512:(fg + 1) * 512, :].rearrange("(j p) c -> p j c", p=128), writes=[an], slot=an)
                    for c in range(NCH):
                        c0 = c * CH
                        ab = (cnt["g"] // 4) % 2
                        for fj in range(4):
                            gb = cnt["g"] % 2
                            cnt["g"] += 1
                            for k in range(8):
                                P.I("pe", "matmul", psG[gb][:], wg_[:, k, fj * 128:(fj + 1) * 128], hT[:, k, c0:c0 + CH], start=(k == 0), stop=(k == 7),
                                    reads=[an, f"hT{k}_{c}"], writes=[f"psG{gb}"])
                            for k in range(8):
                                P.I("pe", "matmul", psU[gb][:], wu_[:, k, fj * 128:(fj + 1) * 128], hT[:, k, c0:c0 + CH], start=(k == 0), stop=(k == 7),
                                    reads=[an, f"hT{k}_{c}"], writes=[f"psU{gb}"])
                            P.I("act", "activation", sb_[gb][:], psG[gb][:], AF.Silu, reads=[f"psG{gb}"], writes=[f"s{gb}"])
                            if moe:
                                P.I("dve", "tensor_tensor", sg_[gb][:], sb_[gb][:], Gb[:, c0:c0 + CH], ALU.mult, reads=[f"s{gb}", f"Gb{c}"], writes=[f"sg{gb}"])
                                src, srcn = sg_[gb], f"sg{gb}"
                            else:
                                src, srcn = sb_[gb], f"s{gb}"
                            P.I("dve", "tensor_tensor", act[ab][:, fj, :], src[:], psU[gb][:], ALU.mult, reads=[srcn, f"psU{gb}"], writes=[f"act{ab}_{fj}"])
                        for dc in range(8):
                            pb = cnt["d"] % 4
                            cnt["d"] += 1
                            for fj in range(4):
                                P.I("pe", "matmul", psD[pb][:], wd_[:, fj, dc * 128:(dc + 1) * 128], act[ab][:, fj, :], start=(fj == 0), stop=(fj == 3),
                                    reads=[an, f"act{ab}_{fj}"], writes=[f"psD{pb}"])
                            P.I("dve", "tensor_tensor", xres[:, dc, c0:c0 + CH], xres[:, dc, c0:c0 + CH], psD[pb][:], ALU.add,
                                reads=[f"xres{dc}_{c}", f"psD{pb}"], writes=[f"xres{dc}_{c}"])
            for c in range(NCH):
                c0 = c * CH
                if final:
                    g = c % 2
                    rms_bcast(lambda k: xres[:, k, c0:c0 + CH], 1024.0, (psS[g], f"psD{2 + g}"), (rs[g], f"rs{g}"), list(range(8)),
                              lambda k: [f"xres{k}_{c}"])
                    for k in range(8):
                        P.I("dve", "scalar_tensor_tensor", xres[:, k, c0:c0 + CH], xres[:, k, c0:c0 + CH], vec[:, 2, k:k + 1], rs[g][:], ALU.mult, ALU.mult,
                            reads=[f"xres{k}_{c}", "vec", f"rs{g}"], writes=[f"xres{k}_{c}"])
                P.dma("sp", out_chunk(blk, c), xres[:, :, c0:c0 + CH], reads=[f"xres{k}_{c}" for k in range(8)], writes=[f"p2out{c}"], slot=f"out{c}")
                if out_done is not None:
                    out_done(blk, c, [f"p2out{c}"])
        print("P2 ops", P.n_ops, "waits", P.n_waits)


def p2_consts():
    c = np.zeros((128, 256 + 1024), np.float32)
    c[:, 0:128] = 1.0
    c[:, 128:256] = np.eye(128)
    for e in range(8):
        c[e, 256 + e * 128: 256 + (e + 1) * 128] = 1.0
    return c


def p2_inputs(xT, yT, layer, W, moe):
    def pk(v):
        return np.ascontiguousarray(v.reshape(8, 128).T)
    gy = np.concatenate([W["lru_out_norm"][layer], W["sb_out_norm"][layer]])
    vec = np.stack([pk(gy), pk(W["ffn_norm"][layer]), pk(W["final_norm"])], axis=1)
    j = layer // 2
    r = {"xT": xT, "yT": yT, "vec": np.ascontiguousarray(vec), "w_out": W["w_out"][layer], "consts": p2_consts()}
    if moe:
        r.update(wg=W["moe_w_gate"][j], wu=W["moe_w_up"][j], wd=W["moe_w_down"][j], w_router=W["router_w"][j])
    else:
        r.update(wg=W["dense_w_gate"][j][None], wu=W["dense_w_up"][j][None], wd=W["dense_w_down"][j][None])
    return r


from concourse.bass_utils import run_bass_kernel_spmd

S_FULL = 8192
T_CORE = 4096
TB_P2 = 2048
PAIRS = [[0, 1], [2, 3], [4, 5], [6, 7]]
_CACHE = {}


def build_fused(S=S_FULL, T=T_CORE, TB=TB_P2):
    nc = bass.Bass("TRN2", target_bir_lowering=False)
    def din(name, shape):
        return nc.dram_tensor(name, list(shape), F32, kind="ExternalInput").ap()
    d = {
        "xT": din("xT", [1024, S]), "xT_own": din("xT_own", [1024, T]),
        "gk1": din("gk1", [2, 128, 8]), "w_in": din("w_in", [2, 1024, 1280]), "conv_w": din("conv_w", [2, 128, 2, 4]),
        "vecs1": din("vecs1", [2, 128, 2, 4]), "w_r": din("w_r", [2, 4, 64, 64]), "w_i": din("w_i", [2, 4, 64, 64]),
        "consts1": din("consts1", [128, 3 * 128 + 4 * 512]),
        "vec2": din("vec2", [2, 128, 3, 8]), "w_out": din("w_out", [2, 1024, 1024]), "consts2": din("consts2", [128, 256 + 1024]),
        "hmask": din("hmask", [128, 2]),
        "dense_wg": din("dense_wg", [1, 1024, 3072]), "dense_wu": din("dense_wu", [1, 1024, 3072]), "dense_wd": din("dense_wd", [1, 3072, 1024]),
        "moe_wg": din("moe_wg", [8, 1024, 3072]), "moe_wu": din("moe_wu", [8, 1024, 3072]), "moe_wd": din("moe_wd", [8, 3072, 1024]),
        "w_router": din("w_router", [1024, 8]),
    }
    outT = nc.dram_tensor("outT", [1024, T], F32, kind="ExternalOutput").ap()
    YP = 1024
    XP = 512
    NYP, NXP = S // YP, T // XP
    ysrc = nc.dram_tensor("ysrc", [NYP, 512, YP], F32).ap()
    ydst = nc.dram_tensor("ydst", [NYP, 1024, YP], F32).ap()
    xsrc = nc.dram_tensor("xsrc", [NXP, 1024, XP], F32).ap()
    xdst = nc.dram_tensor("xdst", [NXP, 2048, XP], F32).ap()
    xT_v = d["xT"].rearrange("(k p) t -> p k t", p=128)
    xo_v = d["xT_own"].rearrange("(k p) t -> p k t", p=128)
    oT_v = outT.rearrange("(k p) t -> p k t", p=128)
    NPC = T // TC

    def ysrc_rows(r0, r1, t0, n):
        return ysrc[t0 // YP][r0:r1, t0 % YP:t0 % YP + n]

    def ych(blk, c):
        o = blk * TB + c * CH
        def piece(tok):
            return ydst[tok // YP].rearrange("(k p) t -> p k t", p=128)[:, :, tok % YP:tok % YP + CH]
        return piece(o), piece(T + o)

    def xs_chunk(blk, c):
        o = blk * TB + c * CH
        return xsrc[o // XP].rearrange("(k p) t -> p k t", p=128)

    def xd_chunk(tc):
        return xdst[tc % NPC].rearrange("(r k p) t -> r p k t", r=2, p=128)[tc // NPC]

    with ExitStack() as st:
        P = Prog(nc, st)
        for L in range(2):
            moe = (L % 2 == 1)
            last = (L == 1)
            P.begin_phase(f"a{L}")
            if L == 0:
                xc = lambda tc: xT_v[:, :, tc * TC:(tc + 1) * TC]
            else:
                xc = xd_chunk
            def y_piece(j, deps):
                P.collective("AllGather", [ysrc[j]], [ydst[j]], PAIRS, reads=deps, writes=[f"ydst{j}"], slot="agy")
            emit_p1(P, S, d, L, xc, ysrc_rows, y_piece, YP // TC)
            P.end_phase()
            P.begin_phase(f"c{L}")
            if L == 0:
                xch = lambda blk, c: xo_v[:, :, blk * TB + c * CH: blk * TB + (c + 1) * CH]
                och = xs_chunk
            else:
                xch = xs_chunk
                och = lambda blk, c: oT_v[:, :, blk * TB + c * CH: blk * TB + (c + 1) * CH]
            def x_piece(blk, c, deps):
                j = (blk * TB + c * CH) // XP
                P.collective("AllGather", [xsrc[j]], [xdst[j]], PAIRS, reads=deps, writes=[f"xdst{j}"], slot="agx")
            emit_p2(P, T, TB, moe, last, d, L, xch, ych, och, None if last else x_piece)
            P.end_phase()
    return nc


def fused_inputs(x, W, c):
    b, hs = c // 2, c % 2
    xTb = np.ascontiguousarray(x[b].T)
    p1 = [p1_inputs_T(None, L, hs, W) for L in range(2)]
    perm = np.concatenate([np.arange(0, 256), np.arange(512, 768), np.arange(256, 512), np.arange(768, 1024)])
    def pk(v):
        return np.ascontiguousarray(v.reshape(8, 128).T)
    vec2 = []
    for L in range(2):
        gy = np.concatenate([W["lru_out_norm"][L], W["sb_out_norm"][L]])[perm]
        vec2.append(np.stack([pk(gy), pk(W["ffn_norm"][L]), pk(W["final_norm"])], axis=1))
    hmask = np.zeros((128, 2), np.float32)
    hmask[:, hs] = 1.0
    return {
        "xT": xTb, "xT_own": np.ascontiguousarray(xTb[:, hs * T_CORE:(hs + 1) * T_CORE]),
        "gk1": np.stack([p["gk"] for p in p1]), "w_in": np.stack([p["w_in"] for p in p1]),
        "conv_w": np.stack([p["conv_w"] for p in p1]), "vecs1": np.stack([p["vecs"] for p in p1]),
        "w_r": np.stack([p["w_r"] for p in p1]), "w_i": np.stack([p["w_i"] for p in p1]),
        "consts1": p1_consts(),
        "vec2": np.ascontiguousarray(np.stack(vec2)), "w_out": np.ascontiguousarray(W["w_out"][:, perm, :]),
        "consts2": p2_consts(), "hmask": hmask,
        "dense_wg": W["dense_w_gate"], "dense_wu": W["dense_w_up"], "dense_wd": W["dense_w_down"],
        "moe_wg": W["moe_w_gate"][0], "moe_wu": W["moe_w_up"][0], "moe_wd": W["moe_w_down"][0],
        "w_router": W["router_w"][0],
    }


def kernel(**inputs):
    W = {k: np.asarray(v, dtype=np.float32) for k, v in inputs.items() if k != "x"}
    x = np.asarray(inputs["x"], dtype=np.float32)
    if "nc" not in _CACHE:
        _CACHE["nc"] = build_fused()
    cores = list(range(8))
    ins = [fused_inputs(x, W, c) for c in cores]
    res = run_bass_kernel_spmd(_CACHE["nc"], ins, core_ids=cores).results
    out = np.empty((4, S_FULL, 1024), np.float32)
    for c in cores:
        b, hs = c // 2, c % 2
        out[b, hs * T_CORE:(hs + 1) * T_CORE, :] = res[c]["outT"].T
    return out
```

```python
from contextlib import ExitStack
import numpy as np
import concourse.bass as bass
import concourse.mybir as mybir

F32 = mybir.dt.float32
BF16 = mybir.dt.bfloat16
U32 = mybir.dt.uint32
AF = mybir.ActivationFunctionType
ALU = mybir.AluOpType
AX = mybir.AxisListType

COMPUTE = ("pe", "act", "dve", "pool")


class Prog:
    def __init__(self, nc, stack):
        self.nc = nc
        self.stack = stack
        self.q = {k: [] for k in ("pe", "act", "dve", "pool", "sp")}
        self.eng = {"pe": nc.tensor, "act": nc.scalar, "dve": nc.vector,
                    "pool": nc.gpsimd, "sp": nc.sync}
        self.sems = {}
        self.count = {}
        self.waited = {}
        self.last_w = {}
        self.readers = {}
        self.n_ops = 0
        self.n_waits = 0
        for k in COMPUTE:
            self._sem("E_" + k)

    def _sem(self, key):
        if key not in self.sems:
            self.sems[key] = self.stack.enter_context(self.nc.semaphore(key))
            self.count[key] = 0
        return self.sems[key]

    def begin_phase(self, tag):
        self.ph = tag
        self.pstack = ExitStack()
        self.pstack.__enter__()

    def end_phase(self):
        self.drain_all()
        self.run()
        self.q = {k: [] for k in self.q}
        self.last_w = {}
        self.readers = {}
        self.pstack.__exit__(None, None, None)

    def drain_all(self):
        for qname in self.q:
            waits = []
            for k, v in self.count.items():
                if v > 0 and self.waited.get((qname, k), 0) < v:
                    self.waited[(qname, k)] = v
                    waits.append((self.sems[k], v))
            def emit(e, waits=waits):
                for s, v in waits:
                    e.wait_ge(s, v)
            self.q[qname].append(emit)

    def sbuf(self, name, shape, dtype):
        return self.pstack.enter_context(self.nc.sbuf_tensor(f"s{self.ph}_" + name, list(shape), dtype))

    def psum(self, name, shape, dtype=F32):
        return self.pstack.enter_context(self.nc.psum_tensor(f"p{self.ph}_" + name, list(shape), dtype))

    def collective(self, kind, ins, outs, groups, reads, writes, slot):
        key = "C_" + slot
        self._sem(key)
        deps = self._deps(reads, writes, skip_key=None)
        waits = self._emit_waits("pool", deps)
        self.count[key] += 1
        val = self.count[key]
        sem = self.sems[key]

        def emit(e, waits=waits, sem=sem):
            for s, v in waits:
                e.wait_ge(s, v)
            e.collective_compute(kind, ALU.bypass, replica_groups=groups, ins=ins, outs=outs).then_inc(sem, 1)
        self.q["pool"].append(emit)
        self._commit(reads, writes, (key, val))

    def _deps(self, reads, writes, skip_key=None):
        deps = {}
        def add(ev):
            if ev is None:
                return
            k, v = ev
            if k == skip_key:
                return
            if deps.get(k, 0) < v:
                deps[k] = v
        for r in reads:
            add(self.last_w.get(r))
        for w in writes:
            add(self.last_w.get(w))
            for ev in self.readers.get(w, ()):
                add(ev)
        return deps

    def _commit(self, reads, writes, ev):
        for r in reads:
            self.readers.setdefault(r, []).append(ev)
        for w in writes:
            self.last_w[w] = ev
            self.readers[w] = []

    def _emit_waits(self, qname, deps):
        out = []
        for k, v in deps.items():
            if self.waited.get((qname, k), 0) >= v:
                continue
            self.waited[(qname, k)] = v
            out.append((self.sems[k], v))
        return out

    def op(self, qname, fn, reads=(), writes=(), pe_acc=False):
        key = "E_" + qname
        deps = self._deps(reads, writes, skip_key=(key if (qname == "pe") else None))
        waits = self._emit_waits(qname, deps)
        self.count[key] += 1
        val = self.count[key]
        sem = self.sems[key]
        self.n_ops += 1
        self.n_waits += len(waits)

        def emit(e, fn=fn, waits=waits, sem=sem):
            for s, v in waits:
                e.wait_ge(s, v)
            fn(e).then_inc(sem, 1)
        self.q[qname].append(emit)
        self._commit(reads, writes, (key, val))

    def I(self, qname, method, *args, reads=(), writes=(), **kw):
        self.op(qname, (lambda e, m=method, a=args, k=kw: getattr(e, m)(*a, **k)), reads, writes)

    def dma(self, qname, out, in_, reads=(), writes=(), slot=None, **kw):
        assert slot is not None
        key = "D_" + slot
        self._sem(key)
        deps = self._deps(reads, writes, skip_key=key)
        waits = self._emit_waits(qname, deps)
        self.count[key] += 16
        val = self.count[key]
        sem = self.sems[key]
        self.n_ops += 1
        self.n_waits += len(waits)

        def emit(e, waits=waits, sem=sem, out=out, in_=in_, kw=kw):
            for s, v in waits:
                e.wait_ge(s, v)
            e.dma_start(out=out, in_=in_, **kw).then_inc(sem, 16)
        self.q[qname].append(emit)
        self._commit(reads, writes, (key, val))

    def finish(self, qname, resources):
        deps = self._deps(resources, resources)
        waits = self._emit_waits(qname, deps)

        def emit(e, waits=waits):
            for s, v in waits:
                e.wait_ge(s, v)
        self.q[qname].append(emit)

    def run(self):
        nc = self.nc
        with nc.Block() as block:
            @block.tensor
            def _(e):
                for f in self.q["pe"]:
                    f(e)

            @block.scalar
            def _(e):
                for f in self.q["act"]:
                    f(e)

            @block.vector
            def _(e):
                for f in self.q["dve"]:
                    f(e)

            @block.gpsimd
            def _(e):
                for f in self.q["pool"]:
                    f(e)

            @block.sync
            def _(e):
                for f in self.q["sp"]:
                    f(e)


NEG = -30000.0
EPS = 1e-6
TC = 512


def emit_p1(P, S, d, L, x_chunk, ysrc, piece_done=None, chunks_per_piece=2):
    nc = P.nc
    NT = S // TC
    NKT = S // 128
    d = {"gk": d["gk1"][L], "w_in": d["w_in"][L], "conv_w": d["conv_w"][L], "vecs": d["vecs1"][L],
         "w_r": d["w_r"][L], "w_i": d["w_i"][L], "consts": d["consts1"]}
    if True:
        if True:
            pass
        cst = P.sbuf("cst", [128, 3 * 128 + 4 * 512], BF16)
        P.dma("pool", cst[:], d["consts"], writes=["cst"], slot="cst")
        ones = cst[:, 0:128]
        trineg = cst[:, 128:256]
        ident = cst[:, 256:384]
        masks = [cst[:, 384 + i * 512: 384 + (i + 1) * 512] for i in range(4)]
        negones = P.sbuf("negones", [128, 128], BF16)
        P.op("dve", lambda e: e.tensor_scalar(negones[:], ones, -1.0, None, ALU.mult), reads=["cst"], writes=["negones"])

        gk = P.sbuf("gk", [128, 8], F32)
        P.dma("sp", gk[:], d["gk"], writes=["gk"], slot="gk")
        convw = P.sbuf("convw", [128, 2, 4], F32)
        P.dma("sp", convw[:], d["conv_w"], writes=["convw"], slot="convw")
        vecs = P.sbuf("vecs", [128, 2, 4], F32)
        P.dma("sp", vecs[:], d["vecs"], writes=["vecs"], slot="vecs")
        c1 = P.sbuf("c1", [128, 2], F32)
        for cc in range(2):
            P.op("act", lambda e, cc=cc: e.activation(c1[:, cc:cc + 1], vecs[:, cc, 3:4], AF.Exp, scale=-1.0), reads=["vecs"], writes=["c1"])
        P.op("act", lambda e: e.activation(c1[:], c1[:], AF.Ln, bias=1.0), reads=["c1"], writes=["c1"])
        P.op("dve", lambda e: e.tensor_scalar(c1[:], c1[:], -8.0, None, ALU.mult), reads=["c1"], writes=["c1"])

        bd_f = P.sbuf("bd_f", [128, 4, 128], F32)
        P.op("pool", lambda e: e.memset(bd_f[:], 0.0), writes=["bd_f"])
        for gi, nm in enumerate(("w_r", "w_i")):
            for cc in range(2):
                for hb in range(2):
                    P.dma("sp", bd_f[hb * 64:(hb + 1) * 64, gi * 2 + cc, hb * 64:(hb + 1) * 64], d[nm][cc * 2 + hb],
                          reads=[], writes=["bd_f"], slot="bd_f")
        bd = P.sbuf("bd", [128, 4, 128], BF16)
        P.op("dve", lambda e: e.tensor_copy(bd[:], bd_f[:]), reads=["bd_f"], writes=["bd"])

        wbf = P.sbuf("wbf", [128, 8, 1280], BF16)
        wst = [P.sbuf(f"wst{i}", [128, 1280], F32) for i in range(2)]
        w_in_v = d["w_in"].rearrange("(k p) c -> p k c", p=128)
        for k in range(8):
            b = k % 2
            P.dma("sp", wst[b][:], w_in_v[:, k, :], writes=[f"wst{b}"], slot=f"wst{b}")
            P.op("dve", lambda e, k=k, b=b: e.tensor_scalar(wbf[:, k, :], wst[b][:], gk[:, k:k + 1], None, ALU.mult),
                 reads=[f"wst{b}", "gk"], writes=["wbf"])
        P.op("dve", lambda e: e.tensor_scalar(wbf[:, :, 512:768], wbf[:, :, 512:768], 0.125, None, ALU.mult), reads=["wbf"], writes=["wbf"])

        kT = P.sbuf("kT", [128, 2, S], BF16)
        V = P.sbuf("V", [128, NKT, 256], BF16)
        xt = P.sbuf("xt", [128, 8, TC], F32)
        sq = P.sbuf("sq", [128, 8, TC], BF16)
        hT = P.sbuf("hT", [128, 8, TC], BF16)
        rstd = P.sbuf("rstd", [128, TC], F32)
        xl = P.sbuf("xl", [128, 2, 3 + TC], F32)
        P.op("pool", lambda e: e.memset(xl[:], 0.0), writes=["xl0", "xl1"])
        xc = P.sbuf("xc", [128, TC], F32)
        xcb = P.sbuf("xcb", [128, TC], BF16)
        rr = P.sbuf("rr", [128, TC], F32)
        ii = P.sbuf("ii", [128, TC], F32)
        ss = P.sbuf("ss", [128, TC], F32)
        hb_ = [P.sbuf(f"hbuf{i}", [128, 2, TC], F32) for i in range(2)]
        gt = P.sbuf("gt", [128, TC], F32)
        t1 = P.sbuf("t1", [128, TC], F32)
        yl = P.sbuf("yl", [128, 2, TC], F32)
        ebuf = [P.sbuf(f"e{i}", [128, 2 * TC], F32) for i in range(2)]
        spb = [P.sbuf(f"sp{i}", [128, 2 * TC], BF16) for i in range(2)]
        spd = [P.sbuf(f"spd{i}", [128, 2 * TC], BF16) for i in range(3)]
        for i in range(3):
            P.I("pool", "memset", spd[i][:], 0.0, writes=[f"spd{i}"])
        wb = [P.sbuf(f"w{i}", [128, 2 * TC], BF16) for i in range(2)]
        Rb = [P.sbuf(f"R{i}", [128, 2 * TC], BF16) for i in range(2)]
        ost = [P.sbuf(f"ost{i}", [128, TC], F32) for i in range(2)]
        qT2 = P.sbuf("qT2", [128, 4, TC], BF16)
        nvecs = P.sbuf("nvecs", [128, 2, 4], F32)
        P.I("dve", "tensor_scalar", nvecs[:], vecs[:], -1.0, None, ALU.mult, reads=["vecs"], writes=["nvecs"])
        psA2 = [P.psum(f"psA{i}", [128, 2 * TC]) for i in range(3)]
        psO1 = P.psum("psO", [128, TC])
        pq_ = P.psum("pq", [128, TC])
        pq = [pq_, pq_]
        npp = [0]

        def prep(tc):
            t0 = tc * TC
            qb = (tc % 2) * 2
            P.dma("sp", xt[:], x_chunk(tc), writes=["xt"], slot="xt")
            P.I("act", "activation", sq[:], xt[:], AF.Square, reads=["xt"], writes=["sq"])
            yield
            for k in range(8):
                P.I("pe", "matmul", pq[0][:], ones, sq[:, k, :], start=(k == 0), stop=(k == 7), reads=["cst", "sq"], writes=["pq0"])
                if k % 4 == 3:
                    yield
            P.I("act", "activation", rstd[:], pq[0][:], AF.Ln, scale=1.0 / 1024, bias=EPS, reads=["pq0"], writes=["rstd"])
            P.I("act", "activation", rstd[:], rstd[:], AF.Exp, scale=-0.5, reads=["rstd"], writes=["rstd"])
            yield
            for k in range(8):
                en = "dve" if k % 2 == 0 else "pool"
                P.I(en, "tensor_tensor", hT[:, k, :], xt[:, k, :], rstd[:], ALU.mult, reads=["xt", "rstd"], writes=[f"hT{k}"])
                if k % 4 == 3:
                    yield
            for cc in range(8):
                pb = npp[0] % 2
                npp[0] += 1
                ps, psn = pq[pb], "pq0"
                for k in range(8):
                    P.I("pe", "matmul", ps[:], wbf[:, k, cc * 128:(cc + 1) * 128], hT[:, k, :], start=(k == 0), stop=(k == 7),
                        reads=["wbf", f"hT{k}"], writes=[psn])
                    if k % 4 == 3:
                        yield
                if cc < 2:
                    P.I("dve", "tensor_copy", xl[:, cc, 3:3 + TC], ps[:], reads=[psn], writes=[f"xl{cc}"])
                elif cc < 4:
                    P.I("dve", "tensor_copy", gt[:], ps[:], reads=[psn], writes=["gt"])
                    yield
                    yield from lru(cc - 2, tc, t0)
                elif cc < 6:
                    P.I("dve", "tensor_copy", qT2[:, qb + cc - 4, :], ps[:], reads=[psn], writes=[f"qT{qb + cc - 4}"])
                else:
                    P.I("dve", "tensor_copy", kT[:, cc - 6, t0:t0 + TC], ps[:], reads=[psn], writes=[f"kT{cc - 6}_{tc}"])
                yield
            for j in range(4):
                kt = tc * 4 + j
                for k in range(8):
                    P.I("pe", "matmul", pq[1][:, 0:256], hT[:, k, j * 128:(j + 1) * 128], wbf[:, k, 1024:1280], start=(k == 0), stop=(k == 7),
                        reads=["wbf", f"hT{k}"], writes=["pq0"])
                    if k % 4 == 3:
                        yield
                P.I("dve", "tensor_copy", V[:, kt, :], pq[1][:, 0:256], reads=["pq0"], writes=[f"V{kt}"])
                yield

        def sigmoid_chain(buf, bn, src_ap, src_names, scale, bias):
            P.I("act", "activation", buf, src_ap, AF.Exp, scale=-scale, bias=bias, reads=src_names, writes=[bn])
            P.I("act", "activation", buf, buf, AF.Ln, bias=1.0, reads=[bn], writes=[bn])
            P.I("act", "activation", buf, buf, AF.Exp, scale=-1.0, reads=[bn], writes=[bn])

        def lru(cc, tc, t0):
            X = f"xl{cc}"
            P.I("dve", "tensor_scalar", xc[:], xl[:, cc, 3:3 + TC], convw[:, cc, 3:4], vecs[:, cc, 0:1], ALU.mult, ALU.add,
                reads=[X, "convw", "vecs"], writes=["xc"])
            yield
            for j in range(3):
                P.I("dve", "scalar_tensor_tensor", xc[:], xl[:, cc, j:j + TC], convw[:, cc, j:j + 1], xc[:], ALU.mult, ALU.add,
                    reads=[X, "convw", "xc"], writes=["xc"])
                yield
            P.I("pool", "tensor_copy", xl[:, cc, 0:3], xl[:, cc, TC:TC + 3], reads=[X], writes=[X])
            P.I("pool", "tensor_copy", xcb[:], xc[:], reads=["xc"], writes=["xcb"])
            yield
            P.I("pe", "matmul", pq[0][:], bd[:, cc, :], xcb[:], start=True, stop=True, reads=["bd", "xcb"], writes=["pq0"])
            yield
            sigmoid_chain(rr[:], "rr", pq[0][:], ["pq0", "nvecs"], 1.0, nvecs[:, cc, 1:2])
            yield
            P.I("pe", "matmul", pq[0][:], bd[:, 2 + cc, :], xcb[:], start=True, stop=True, reads=["bd", "xcb"], writes=["pq0"])
            yield
            sigmoid_chain(ii[:], "ii", pq[0][:], ["pq0", "nvecs"], 1.0, nvecs[:, cc, 2:3])
            yield
            P.I("act", "activation", rr[:], rr[:], AF.Exp, scale=c1[:, cc:cc + 1], reads=["rr", "c1"], writes=["rr"])
            P.I("dve", "scalar_tensor_tensor", ss[:], rr[:], 0.9999999, rr[:], ALU.min, ALU.mult, reads=["rr"], writes=["ss"])
            yield
            P.I("act", "activation", ss[:], ss[:], AF.Ln, scale=-1.0, bias=1.0, reads=["ss"], writes=["ss"])
            P.I("act", "activation", ss[:], ss[:], AF.Exp, scale=0.5, reads=["ss"], writes=["ss"])
            P.I("dve", "tensor_tensor", ii[:], ii[:], xc[:], ALU.mult, reads=["ii", "xc"], writes=["ii"])
            yield
            P.I("dve", "tensor_tensor", ii[:], ii[:], ss[:], ALU.mult, reads=["ii", "ss"], writes=["ii"])
            hcur, hprev = hb_[tc % 2], hb_[(tc + 1) % 2]
            Hc, Hp = f"h{tc % 2}_{cc}", f"h{(tc + 1) % 2}_{cc}"
            yield
            if tc == 0:
                P.I("dve", "tensor_tensor_scan", hcur[:, cc, :], rr[:], ii[:], 0.0, ALU.mult, ALU.add, reads=["rr", "ii"], writes=[Hc])
            else:
                P.I("dve", "tensor_tensor_scan", hcur[:, cc, :], rr[:], ii[:], hprev[:, cc, TC - 1:TC], ALU.mult, ALU.add,
                    reads=["rr", "ii", Hp], writes=[Hc])
            yield
            P.I("pool", "tensor_tensor", t1[:], gt[:], gt[:], ALU.mult, reads=["gt"], writes=["t1"])
            P.I("dve", "tensor_scalar", t1[:], t1[:], 0.044715, 1.0, ALU.mult, ALU.add, reads=["t1"], writes=["t1"])
            yield
            P.I("pool", "tensor_tensor", t1[:], t1[:], gt[:], ALU.mult, reads=["t1", "gt"], writes=["t1"])
            sigmoid_chain(t1[:], "t1", t1[:], ["t1"], 1.5957691216057308, 0.0)
            yield
            P.I("pool", "tensor_tensor", t1[:], t1[:], gt[:], ALU.mult, reads=["t1", "gt"], writes=["t1"])
            P.I("dve", "tensor_tensor", yl[:, cc, :], hcur[:, cc, :], t1[:], ALU.mult, reads=[Hc, "t1"], writes=[f"yl{cc}"])
            P.dma("sp", ysrc(cc * 128, (cc + 1) * 128, t0, TC), yl[:, cc, :], reads=[f"yl{cc}"], writes=[f"ysrc{tc // chunks_per_piece}_yl{cc}"], slot=f"yl{cc}")
            yield

        def attention(tc, nxt, n_units):
            t0 = tc * TC
            qb = (tc % 2) * 2
            nk = tc * 4 + 4
            steps_left = 2 * (nk + 1)
            units_left = n_units if nxt is not None else 0
            An = lambda s: [f"A{s}h0", f"A{s}h1"]
            for hpair in range(2):
                hc = hpair

                def qk(n):
                    kt = nk - 1 - n
                    s = n % 3
                    diag = kt >= tc * 4
                    for hh in range(2):
                        hp = hh * 64
                        P.I("pe", "matmul", psA2[s][:, hh * TC:(hh + 1) * TC], kT[hp:hp + 64, hc, kt * 128:(kt + 1) * 128], qT2[hp:hp + 64, qb + hc, :],
                            start=True, stop=(not diag), reads=[f"kT{hc}_{kt // 4}", f"qT{qb + hc}"], writes=[f"A{s}h{hh}"])
                    if diag:
                        for hh in range(2):
                            P.I("pe", "matmul", psA2[s][:, hh * TC:(hh + 1) * TC], ident, masks[kt - tc * 4], start=False, stop=True,
                                reads=["cst"], writes=[f"A{s}h{hh}"])

                def spbuf(n):
                    return (spd[n], f"spd{n}") if n < 3 else (spb[n % 2], f"sp{n % 2}")

                def cum(n):
                    s, b = n % 3, n % 2
                    spt, spn = spbuf(n)
                    for hh in range(2):
                        P.I("pe", "matmul", psA2[s][:, hh * TC:(hh + 1) * TC], trineg, spt[:, hh * TC:(hh + 1) * TC], start=False, stop=(n == 0),
                            skip_group_check=True, reads=["cst", spn], writes=[f"A{s}h{hh}"])
                    if n > 0:
                        for hh in range(2):
                            P.I("pe", "matmul", psA2[s][:, hh * TC:(hh + 1) * TC], negones[:], Rb[1 - b][:, hh * TC:(hh + 1) * TC], start=False, stop=True,
                                skip_group_check=True, reads=["negones", f"R{1 - b}"], writes=[f"A{s}h{hh}"])
                        P.I("dve", "tensor_tensor", Rb[b][:], Rb[1 - b][:], spt[:], ALU.add, reads=[f"R{1 - b}", spn], writes=[f"R{b}"])
                    else:
                        P.I("dve", "tensor_copy", Rb[b][:], spt[:], reads=[spn], writes=[f"R{b}"])

                def wv(n):
                    kt = nk - 1 - n
                    b = n % 2
                    for hh in range(2):
                        h = 2 * hpair + hh
                        P.I("pe", "matmul", psO1[hh * 64:(hh + 1) * 64, :], V[:, kt, h * 64:(h + 1) * 64], wb[b][:, hh * TC:(hh + 1) * TC],
                            start=(n == 0), stop=(n == nk - 1), reads=[f"V{kt}", f"w{b}"], writes=[f"O{hh}"])

                qk(0)
                qk(1)
                for n in range(nk + 1):
                    if n < nk:
                        s, b = n % 3, n % 2
                        spt, spn = spbuf(n)
                        lo = (3 - n) * 128 if n < 3 else 0
                        v3 = lambda ap: ap if lo == 0 else ap.rearrange("p (h t) -> p h t", h=2)[:, :, lo:TC]
                        P.I("act", "activation", v3(ebuf[b][:]), v3(psA2[s][:]), AF.Exp, reads=An(s), writes=[f"e{b}"])
                        P.I("act", "activation", v3(spt[:]), v3(ebuf[b][:]), AF.Ln, bias=1.0, reads=[f"e{b}"], writes=[spn])
                        cum(n)
                    if n >= 1:
                        s1, b1 = (n - 1) % 3, (n - 1) % 2
                        P.I("act", "activation", wb[b1][:], psA2[s1][:], AF.Exp, reads=An(s1), writes=[f"w{b1}"])
                        wv(n - 1)
                    if n + 2 < nk:
                        qk(n + 2)
                    if nxt is not None and units_left > 0:
                        take = -(-units_left // steps_left)
                        for _ in range(take):
                            if next(nxt, "done") == "done":
                                units_left = 0
                                break
                            units_left -= 1
                    steps_left -= 1
                os_ = ost[hpair]
                P.I("dve", "tensor_copy", os_[:], psO1[:], reads=["O0", "O1"], writes=[f"ost{hpair}"])
                P.dma("sp", ysrc(256 + hpair * 128, 256 + (hpair + 1) * 128, t0, TC), os_[:], reads=[f"ost{hpair}"], writes=[f"ysrc{tc // chunks_per_piece}_ost{hpair}"], slot=f"ost{hpair}")
            if nxt is not None:
                for _ in nxt:
                    pass

        n_units = sum(1 for _ in prep(0))
        for tc in range(NT):
            attention(tc, prep(tc + 1) if tc + 1 < NT else None, n_units)
            if piece_done is not None and tc % chunks_per_piece == chunks_per_piece - 1:
                j = tc // chunks_per_piece
                piece_done(j, [f"ysrc{j}_yl0", f"ysrc{j}_yl1", f"ysrc{j}_ost0", f"ysrc{j}_ost1"])
        print("P1 ops", P.n_ops, "waits", P.n_waits)


def p1_consts():
    c = np.zeros((128, 3 * 128 + 4 * 512), np.float32)
    c[:, 0:128] = 1.0
    j = np.arange(128)[:, None]
    s = np.arange(128)[None, :]
    c[:, 128:256] = np.where(j >= s, -1.0, 0.0)
    c[:, 256:384] = np.eye(128)
    t = np.arange(512)[None, :]
    for ki in range(4):
        c[:, 384 + ki * 512: 384 + (ki + 1) * 512] = np.where((ki * 128 + j) < t, 0.0, NEG)
    return c


def p1_inputs_T(xTb, layer, hs, W):
    sl = slice(hs * 256, (hs + 1) * 256)
    w_in = W["w_in"][layer]
    cols = np.concatenate([w_in[:, 0:512][:, sl], w_in[:, 512:1024][:, sl], w_in[:, 1024:1536][:, sl],
                           w_in[:, 1536:2048][:, sl], w_in[:, 2048:2560][:, sl]], axis=1)
    def pc(v):
        return np.ascontiguousarray(v[sl].reshape(2, 128).T)
    conv_w = np.ascontiguousarray(W["conv_w"][layer][:, sl].reshape(4, 2, 128).transpose(2, 1, 0))
    vecs = np.stack([pc(W["conv_b"][layer]), pc(W["b_rgate"][layer].reshape(-1)), pc(W["b_igate"][layer].reshape(-1)),
                     pc(W["lru_lambda"][layer])], axis=2)
    return {
        "gk": np.ascontiguousarray(W["mix_norm"][layer].reshape(8, 128).T),
        "w_in": np.ascontiguousarray(cols),
        "conv_w": conv_w,
        "vecs": np.ascontiguousarray(vecs),
        "w_r": np.ascontiguousarray(W["w_rgate"][layer][hs * 4:(hs + 1) * 4]),
        "w_i": np.ascontiguousarray(W["w_igate"][layer][hs * 4:(hs + 1) * 4]),
        "consts": p1_consts(),
    }


CH = 512


def emit_p2(P, T, TB, moe, final, d, L, x_chunk, y_chunks, out_chunk, out_done=None):
    nc = P.nc
    NB = T // TB
    NCH = TB // CH
    NE = 8 if moe else 1
    j = L // 2
    d = {"vec": d["vec2"][L], "w_out": d["w_out"][L], "consts": d["consts2"], "hmask": d["hmask"],
         "wg": d["moe_wg"] if moe else d["dense_wg"], "wu": d["moe_wu"] if moe else d["dense_wu"],
         "wd": d["moe_wd"] if moe else d["dense_wd"], "w_router": d["w_router"]}
    LRU_K = (0, 1, 4, 5)
    SB_K = (2, 3, 6, 7)
    if True:
        if True:
            pass
        cstf = P.sbuf("cstf", [128, 256 + 1024], F32)
        P.dma("sp", cstf[:], d["consts"], writes=["cstf"], slot="cstf")
        cstb = P.sbuf("cstb", [128, 128], BF16)
        P.I("dve", "tensor_copy", cstb[:], cstf[:, 0:128], reads=["cstf"], writes=["cstb"])
        ones = cstb[:, 0:128]
        onesf = cstf[:, 0:128]
        identf = cstf[:, 128:256]
        sel = [cstf[0:8, 256 + e * 128: 256 + (e + 1) * 128] for e in range(8)]
        vec = P.sbuf("vec", [128, 3, 8], F32)
        P.dma("sp", vec[:], d["vec"], writes=["vec"], slot="vec")

        hmask = P.sbuf("hmask", [128, 2], F32)
        P.dma("sp", hmask[:], d["hmask"], writes=["hmask"], slot="vec")
        xres = P.sbuf("xres", [128, 8, TB], F32)
        hT = P.sbuf("hT", [128, 8, TB], BF16)
        arena = P.sbuf("arena", [128, 24576], BF16)
        def wview(b, which):
            base = b * 12288
            if which == "g":
                return arena[:, base:base + 4096].rearrange("p (k f) -> p k f", k=8)
            if which == "u":
                return arena[:, base + 4096:base + 8192].rearrange("p (k f) -> p k f", k=8)
            return arena[:, base + 8192:base + 12288].rearrange("p (j d) -> p j d", j=4)
        wout = arena[:, 0:8192].rearrange("p (k c) -> p k c", k=8)
        yst = P.sbuf("yst", [128, 8, CH], F32)
        yst2 = arena[:, 12288:12288 + 8192].bitcast(F32).rearrange("p (k t) -> p k t", k=8)
        sq = P.sbuf("sq", [128, 8, CH], BF16)
        rs = [P.sbuf(f"rs{i}", [128, CH], F32) for i in range(2)]
        sb_ = [P.sbuf(f"s{i}", [128, CH], BF16) for i in range(2)]
        sg_ = [P.sbuf(f"sg{i}", [128, CH], BF16) for i in range(2)]
        act = [P.sbuf(f"act{i}", [128, 4, CH], BF16) for i in range(2)]
        psG = [P.psum(f"psG{i}", [128, CH]) for i in range(2)]
        psU = [P.psum(f"psU{i}", [128, CH]) for i in range(2)]
        psD = [P.psum(f"psD{i}", [128, CH]) for i in range(4)]
        psS = [psD[2], psD[3]]
        if moe:
            wr = P.sbuf("wr", [128, 8, 8], F32)
            P.dma("sp", wr[:], d["w_router"].rearrange("(k p) e -> p k e", p=128), writes=["wr"], slot="wr")
            for k in range(8):
                P.I("dve", "tensor_scalar", wr[:, k, :], wr[:, k, :], vec[:, 1, k:k + 1], None, ALU.mult, reads=["wr", "vec"], writes=["wr"])
            GT = P.sbuf("GT", [8, TB], F32)
            Gb = P.sbuf("Gb", [128, TB], BF16)
            lg = P.sbuf("lg", [128, 8], F32)
            mx = P.sbuf("mx", [128, 8], F32)
            gsm = P.sbuf("gsm", [128, 4], F32)
            m1 = P.sbuf("m1", [128, 8], F32)
            m2 = P.sbuf("m2", [128, 8], F32)
            rtok = P.sbuf("rtok", [128, 2], F32)

        cnt = {"g": 0, "d": 0}
        wcnt = [0]

        def rms_bcast(src_k, scale_n, ps, out_rs, kr, src_names):
            for k in kr:
                P.I("act", "activation", sq[:, k, :], src_k(k), AF.Square, reads=src_names(k), writes=[f"sq{k}"])
            for n, k in enumerate(kr):
                P.I("pe", "matmul", ps[0][:], ones, sq[:, k, :], start=(n == 0), stop=(n == len(kr) - 1),
                    reads=["cstb", f"sq{k}"], writes=[ps[1]])
            P.I("act", "activation", out_rs[0][:], ps[0][:], AF.Ln, scale=1.0 / scale_n, bias=EPS, reads=[ps[1]], writes=[out_rs[1]])
            P.I("act", "activation", out_rs[0][:], out_rs[0][:], AF.Exp, scale=-0.5, reads=[out_rs[1]], writes=[out_rs[1]])

        for blk in range(NB):
            b0 = blk * TB
            P.dma("pool", wout, d["w_out"].rearrange("(k p) c -> p k c", p=128), writes=["arena0"], slot="arena0")
            for c in range(NCH):
                P.dma("sp", xres[:, :, c * CH:(c + 1) * CH], x_chunk(blk, c), writes=[f"xres{k}_{c}" for k in range(8)], slot=f"xres{c}")
            for c in range(NCH):
                c0 = c * CH
                yA, yB = y_chunks(blk, c)
                P.dma("sp", yst[:], yA, writes=[f"yst{k}" for k in range(8)], slot="yst")
                P.dma("sp", yst2, yB, writes=["arena1"], slot="yst2")
                for k in range(8):
                    P.I("dve", "tensor_scalar", yst[:, k, :], yst[:, k, :], hmask[:, 0:1], None, ALU.mult, reads=[f"yst{k}", "hmask"], writes=[f"yst{k}"])
                    P.I("dve", "scalar_tensor_tensor", yst[:, k, :], yst2[:, k, :], hmask[:, 1:2], yst[:, k, :], ALU.mult, ALU.add,
                        reads=["arena1", f"yst{k}", "hmask"], writes=[f"yst{k}"])
                for g in range(2):
                    kr = list(LRU_K if g == 0 else SB_K)
                    rms_bcast(lambda k: yst[:, k, :], 512.0, (psS[g], f"psD{2 + g}"), (rs[g], f"rs{g}"), kr, lambda k: [f"yst{k}"])
                    for k in kr:
                        P.I("dve", "scalar_tensor_tensor", hT[:, k, c0:c0 + CH], yst[:, k, :], vec[:, 0, k:k + 1], rs[g][:], ALU.mult, ALU.mult,
                            reads=[f"yst{k}", "vec", f"rs{g}"], writes=[f"hT{k}_{c}"])
                for dc in range(8):
                    pb = cnt["d"] % 4
                    cnt["d"] += 1
                    for k in range(8):
                        P.I("pe", "matmul", psD[pb][:], wout[:, k, dc * 128:(dc + 1) * 128], hT[:, k, c0:c0 + CH], start=(k == 0), stop=(k == 7),
                            reads=["arena0", f"hT{k}_{c}"], writes=[f"psD{pb}"])
                    P.I("dve", "tensor_tensor", xres[:, dc, c0:c0 + CH], xres[:, dc, c0:c0 + CH], psD[pb][:], ALU.add,
                        reads=[f"xres{dc}_{c}", f"psD{pb}"], writes=[f"xres{dc}_{c}"])
            for c in range(NCH):
                c0 = c * CH
                g = c % 2
                rms_bcast(lambda k: xres[:, k, c0:c0 + CH], 1024.0, (psS[g], f"psD{2 + g}"), (rs[g], f"rs{g}"), list(range(8)),
                          lambda k: [f"xres{k}_{c}"])
                for k in range(8):
                    P.I("dve", "scalar_tensor_tensor", hT[:, k, c0:c0 + CH], xres[:, k, c0:c0 + CH], vec[:, 1, k:k + 1], rs[g][:], ALU.mult, ALU.mult,
                        reads=[f"xres{k}_{c}", "vec", f"rs{g}"], writes=[f"hT{k}_{c}"])
                if moe:
                    for j in range(4):
                        tt = c0 + j * 128
                        pg = psG[j % 2]
                        pgn = f"psG{j % 2}"
                        for k in range(8):
                            P.I("pe", "matmul", pg[:, 0:8], xres[:, k, tt:tt + 128], wr[:, k, :], start=(k == 0), stop=(k == 7),
                                reads=[f"xres{k}_{c}", "wr"], writes=[pgn])
                        for k in range(8):
                            P.I("pe", "matmul", pg[:, 8:9], sq[:, k, j * 128:(j + 1) * 128], cstb[:, 0:1], start=(k == 0), stop=(k == 7),
                                reads=[f"sq{k}", "cstb"], writes=[pgn], skip_group_check=True)
                        P.I("act", "activation", rtok[:, 0:1], pg[:, 8:9], AF.Sqrt, scale=1.0 / 1024, bias=EPS, reads=[pgn], writes=["rtok"])
                        P.I("dve", "reciprocal", rtok[:, 1:2], rtok[:, 0:1], reads=["rtok"], writes=["rtok"])
                        P.I("dve", "tensor_scalar", lg[:], pg[:, 0:8], rtok[:, 1:2], None, ALU.mult, reads=[pgn, "rtok"], writes=["lg"])
                        P.I("dve", "max", mx[:], lg[:], reads=["lg"], writes=["mx"])
                        P.I("dve", "tensor_tensor", gsm[:, 0:1], mx[:, 0:1], mx[:, 1:2], ALU.subtract, reads=["mx"], writes=["gsm"])
                        P.I("act", "activation", gsm[:, 1:2], gsm[:, 0:1], AF.Sigmoid, reads=["gsm"], writes=["gsm"])
                        P.I("dve", "tensor_scalar", gsm[:, 2:3], gsm[:, 1:2], -1.0, 1.0, ALU.mult, ALU.add, reads=["gsm"], writes=["gsm"])
                        P.I("dve", "tensor_scalar", m1[:], lg[:], mx[:, 0:1], gsm[:, 1:2], ALU.is_equal, ALU.mult, reads=["lg", "mx", "gsm"], writes=["m1"])
                        P.I("dve", "tensor_scalar", m2[:], lg[:], mx[:, 1:2], gsm[:, 2:3], ALU.is_equal, ALU.mult, reads=["lg", "mx", "gsm"], writes=["m2"])
                        P.I("dve", "tensor_tensor", m1[:], m1[:], m2[:], ALU.add, reads=["m1", "m2"], writes=["m1"])
                        pu = psU[j % 2]
                        pun = f"psU{j % 2}"
                        P.I("pe", "transpose", pu[0:8, 0:128], m1[:], identf, reads=["m1", "cstf"], writes=[pun])
                        P.I("dve", "tensor_copy", GT[:, tt:tt + 128], pu[0:8, 0:128], reads=[pun], writes=[f"GT{c}"])
            for e in range(NE):
                if moe:
                    for c in range(NCH):
                        c0 = c * CH
                        pb = cnt["d"] % 4
                        cnt["d"] += 1
                        P.I("pe", "matmul", psD[pb][:], sel[e], GT[:, c0:c0 + CH], start=True, stop=True, reads=["cstf", f"GT{c}"], writes=[f"psD{pb}"])
                        P.I("act", "activation", Gb[:, c0:c0 + CH], psD[pb][:], AF.Copy, reads=[f"psD{pb}"], writes=[f"Gb{c}"])
                for fg in range(6):
                    wb = wcnt[0] % 2
                    wcnt[0] += 1
                    wg_, wu_, wd_ = wview(wb, "g"), wview(wb, "u"), wview(wb, "d")
                    an = f"arena{wb}"
                    P.dma("pool", wg_, d["wg"][e].rearrange("(k p) f -> p k f", p=128)[:, :, fg * 512:(fg + 1) * 512], writes=[an], slot=an)
                    P.dma("pool", wu_, d["wu"][e].rearrange("(k p) f -> p k f", p=128)[:, :, fg * 512:(fg + 1) * 512], writes=[an], slot=an)
                    P.dma("pool", wd_, d["wd"][e][fg * 512:(fg + 1) * 512, :].rearrange("(j p) c -> p j c", p=128), writes=[an], slot=an)
                    for c in range(NCH):
                        c0 = c * CH
                        ab = (cnt["g"] // 4) % 2
                        for fj in range(4):
                            gb = cnt["g"] % 2
                            cnt["g"] += 1
                            for k in range(8):
                                P.I("pe", "matmul", psG[gb][:], wg_[:, k, fj * 128:(fj + 1) * 128], hT[:, k, c0:c0 + CH], start=(k == 0), stop=(k == 7),
                                    reads=[an, f"hT{k}_{c}"], writes=[f"psG{gb}"])
                            for k in range(8):
                                P.I("pe", "matmul", psU[gb][:], wu_[:, k, fj * 128:(fj + 1) * 128], hT[:, k, c0:c0 + CH], start=(k == 0), stop=(k == 7),
                                    reads=[an, f"hT{k}_{c}"], writes=[f"psU{gb}"])
                            P.I("act", "activation", sb_[gb][:], psG[gb][:], AF.Silu, reads=[f"psG{gb}"], writes=[f"s{gb}"])
                            if moe:
                                P.I("dve", "tensor_tensor", sg_[gb][:], sb_[gb][:], Gb[:, c0:c0 + CH], ALU.mult, reads=[f"s{gb}", f"Gb{c}"], writes=[f"sg{gb}"])
                                src, srcn = sg_[gb], f"sg{gb}"
                            else:
                                src, srcn = sb_[gb], f"s{gb}"
                            P.I("dve", "tensor_tensor", act[ab][:, fj, :], src[:], psU[gb][:], ALU.mult, reads=[srcn, f"psU{gb}"], writes=[f"act{ab}_{fj}"])
                        for dc in range(8):
                            pb = cnt["d"] % 4
                            cnt["d"] += 1
                            for fj in range(4):
                                P.I("pe", "matmul", psD[pb][:], wd_[:, fj, dc * 128:(dc + 1) * 128], act[ab][:, fj, :], start=(fj == 0), stop=(fj == 3),
                                    reads=[an, f"act{ab}_{fj}"], writes=[f"psD{pb}"])
                            P.I("dve", "tensor_tensor", xres[:, dc, c0:c0 + CH], xres[:, dc, c0:c0 + CH], psD[pb][:], ALU.add,
                                reads=[f"xres{dc}_{c}", f"psD{pb}"], writes=[f"xres{dc}_{c}"])
            for c in range(NCH):
                c0 = c * CH
                if final:
                    g = c % 2
                    rms_bcast(lambda k: xres[:, k, c0:c0 + CH], 1024.0, (psS[g], f"psD{2 + g}"), (rs[g], f"rs{g}"), list(range(8)),
                              lambda k: [f"xres{k}_{c}"])
                    for k in range(8):
                        P.I("dve", "scalar_tensor_tensor", xres[:, k, c0:c0 + CH], xres[:, k, c0:c0 + CH], vec[:, 2, k:k + 1], rs[g][:], ALU.mult, ALU.mult,
                            reads=[f"xres{k}_{c}", "vec", f"rs{g}"], writes=[f"xres{k}_{c}"])
                P.dma("sp", out_chunk(blk, c), xres[:, :, c0:c0 + CH], reads=[f"xres{k}_{c}" for k in range(8)], writes=[f"p2out{c}"], slot=f"out{c}")
                if out_done is not None:
                    out_done(blk, c, [f"p2out{c}"])
        print("P2 ops", P.n_ops, "waits", P.n_waits)


def p2_consts():
    c = np.zeros((128, 256 + 1024), np.float32)
    c[:, 0:128] = 1.0
    c[:, 128:256] = np.eye(128)
    for e in range(8):
        c[e, 256 + e * 128: 256 + (e + 1) * 128] = 1.0
    return c


def p2_inputs(xT, yT, layer, W, moe):
    def pk(v):
        return np.ascontiguousarray(v.reshape(8, 128).T)
    gy = np.concatenate([W["lru_out_norm"][layer], W["sb_out_norm"][layer]])
    vec = np.stack([pk(gy), pk(W["ffn_norm"][layer]), pk(W["final_norm"])], axis=1)
    j = layer // 2
    r = {"xT": xT, "yT": yT, "vec": np.ascontiguousarray(vec), "w_out": W["w_out"][layer], "consts": p2_consts()}
    if moe:
        r.update(wg=W["moe_w_gate"][j], wu=W["moe_w_up"][j], wd=W["moe_w_down"][j], w_router=W["router_w"][j])
    else:
        r.update(wg=W["dense_w_gate"][j][None], wu=W["dense_w_up"][j][None], wd=W["dense_w_down"][j][None])
    return r


from concourse.bass_utils import run_bass_kernel_spmd

S_FULL = 8192
T_CORE = 4096
TB_P2 = 2048
PAIRS = [[0, 1], [2, 3], [4, 5], [6, 7]]
_CACHE = {}


def build_fused(S=S_FULL, T=T_CORE, TB=TB_P2):
    nc = bass.Bass("TRN2", target_bir_lowering=False)
    def din(name, shape):
        return nc.dram_tensor(name, list(shape), F32, kind="ExternalInput").ap()
    d = {
        "xT": din("xT", [1024, S]), "xT_own": din("xT_own", [1024, T]),
        "gk1": din("gk1", [2, 128, 8]), "w_in": din("w_in", [2, 1024, 1280]), "conv_w": din("conv_w", [2, 128, 2, 4]),
        "vecs1": din("vecs1", [2, 128, 2, 4]), "w_r": din("w_r", [2, 4, 64, 64]), "w_i": din("w_i", [2, 4, 64, 64]),
        "consts1": din("consts1", [128, 3 * 128 + 4 * 512]),
        "vec2": din("vec2", [2, 128, 3, 8]), "w_out": din("w_out", [2, 1024, 1024]), "consts2": din("consts2", [128, 256 + 1024]),
        "hmask": din("hmask", [128, 2]),
        "dense_wg": din("dense_wg", [1, 1024, 3072]), "dense_wu": din("dense_wu", [1, 1024, 3072]), "dense_wd": din("dense_wd", [1, 3072, 1024]),
        "moe_wg": din("moe_wg", [8, 1024, 3072]), "moe_wu": din("moe_wu", [8, 1024, 3072]), "moe_wd": din("moe_wd", [8, 3072, 1024]),
        "w_router": din("w_router", [1024, 8]),
    }
    outT = nc.dram_tensor("outT", [1024, T], F32, kind="ExternalOutput").ap()
    YP = 1024
    XP = 512
    NYP, NXP = S // YP, T // XP
    ysrc = nc.dram_tensor("ysrc", [NYP, 512, YP], F32).ap()
    ydst = nc.dram_tensor("ydst", [NYP, 1024, YP], F32).ap()
    xsrc = nc.dram_tensor("xsrc", [NXP, 1024, XP], F32).ap()
    xdst = nc.dram_tensor("xdst", [NXP, 2048, XP], F32).ap()
    xT_v = d["xT"].rearrange("(k p) t -> p k t", p=128)
    xo_v = d["xT_own"].rearrange("(k p) t -> p k t", p=128)
    oT_v = outT.rearrange("(k p) t -> p k t", p=128)
    NPC = T // TC

    def ysrc_rows(r0, r1, t0, n):
        return ysrc[t0 // YP][r0:r1, t0 % YP:t0 % YP + n]

    def ych(blk, c):
        o = blk * TB + c * CH
        def piece(tok):
            return ydst[tok // YP].rearrange("(k p) t -> p k t", p=128)[:, :, tok % YP:tok % YP + CH]
        return piece(o), piece(T + o)

    def xs_chunk(blk, c):
        o = blk * TB + c * CH
        return xsrc[o // XP].rearrange("(k p) t -> p k t", p=128)

    def xd_chunk(tc):
        return xdst[tc % NPC].rearrange("(r k p) t -> r p k t", r=2, p=128)[tc // NPC]

    with ExitStack() as st:
        P = Prog(nc, st)
        for L in range(2):
            moe = (L % 2 == 1)
            last = (L == 1)
            P.begin_phase(f"a{L}")
            if L == 0:
                xc = lambda tc: xT_v[:, :, tc * TC:(tc + 1) * TC]
            else:
                xc = xd_chunk
            def y_piece(j, deps):
                P.collective("AllGather", [ysrc[j]], [ydst[j]], PAIRS, reads=deps, writes=[f"ydst{j}"], slot="agy")
            emit_p1(P, S, d, L, xc, ysrc_rows, y_piece, YP // TC)
            P.end_phase()
            P.begin_phase(f"c{L}")
            if L == 0:
                xch = lambda blk, c: xo_v[:, :, blk * TB + c * CH: blk * TB + (c + 1) * CH]
                och = xs_chunk
            else:
                xch = xs_chunk
                och = lambda blk, c: oT_v[:, :, blk * TB + c * CH: blk * TB + (c + 1) * CH]
            def x_piece(blk, c, deps):
                j = (blk * TB + c * CH) // XP
                P.collective("AllGather", [xsrc[j]], [xdst[j]], PAIRS, reads=deps, writes=[f"xdst{j}"], slot="agx")
            emit_p2(P, T, TB, moe, last, d, L, xch, ych, och, None if last else x_piece)
            P.end_phase()
    return nc


def fused_inputs(x, W, c):
    b, hs = c // 2, c % 2
    xTb = np.ascontiguousarray(x[b].T)
    p1 = [p1_inputs_T(None, L, hs, W) for L in range(2)]
    perm = np.concatenate([np.arange(0, 256), np.arange(512, 768), np.arange(256, 512), np.arange(768, 1024)])
    def pk(v):
        return np.ascontiguousarray(v.reshape(8, 128).T)
    vec2 = []
    for L in range(2):
        gy = np.concatenate([W["lru_out_norm"][L], W["sb_out_norm"][L]])[perm]
        vec2.append(np.stack([pk(gy), pk(W["ffn_norm"][L]), pk(W["final_norm"])], axis=1))
    hmask = np.zeros((128, 2), np.float32)
    hmask[:, hs] = 1.0
    return {
        "xT": xTb, "xT_own": np.ascontiguousarray(xTb[:, hs * T_CORE:(hs + 1) * T_CORE]),
        "gk1": np.stack([p["gk"] for p in p1]), "w_in": np.stack([p["w_in"] for p in p1]),
        "conv_w": np.stack([p["conv_w"] for p in p1]), "vecs1": np.stack([p["vecs"] for p in p1]),
        "w_r": np.stack([p["w_r"] for p in p1]), "w_i": np.stack([p["w_i"] for p in p1]),
        "consts1": p1_consts(),
        "vec2": np.ascontiguousarray(np.stack(vec2)), "w_out": np.ascontiguousarray(W["w_out"][:, perm, :]),
        "consts2": p2_consts(), "hmask": hmask,
        "dense_wg": W["dense_w_gate"], "dense_wu": W["dense_w_up"], "dense_wd": W["dense_w_down"],
        "moe_wg": W["moe_w_gate"][0], "moe_wu": W["moe_w_up"][0], "moe_wd": W["moe_w_down"][0],
        "w_router": W["router_w"][0],
    }


def kernel(**inputs):
    W = {k: np.asarray(v, dtype=np.float32) for k, v in inputs.items() if k != "x"}
    x = np.asarray(inputs["x"], dtype=np.float32)
    if "nc" not in _CACHE:
        _CACHE["nc"] = build_fused()
    cores = list(range(8))
    ins = [fused_inputs(x, W, c) for c in cores]
    res = run_bass_kernel_spmd(_CACHE["nc"], ins, core_ids=cores).results
    out = np.empty((4, S_FULL, 1024), np.float32)
    for c in cores:
        b, hs = c // 2, c % 2
        out[b, hs * T_CORE:(hs + 1) * T_CORE, :] = res[c]["outT"].T
    return out
```

```python
from contextlib import ExitStack
import numpy as np
import concourse.bass as bass
import concourse.mybir as mybir

F32 = mybir.dt.float32
BF16 = mybir.dt.bfloat16
U32 = mybir.dt.uint32
AF = mybir.ActivationFunctionType
ALU = mybir.AluOpType
AX = mybir.AxisListType

COMPUTE = ("pe", "act", "dve", "pool")


class Prog:
    def __init__(self, nc, stack):
        self.nc = nc
        self.stack = stack
        self.q = {k: [] for k in ("pe", "act", "dve", "pool", "sp")}
        self.eng = {"pe": nc.tensor, "act": nc.scalar, "dve": nc.vector,
                    "pool": nc.gpsimd, "sp": nc.sync}
        self.sems = {}
        self.count = {}
        self.waited = {}
        self.last_w = {}
        self.readers = {}
        self.n_ops = 0
        self.n_waits = 0
        for k in COMPUTE:
            self._sem("E_" + k)

    def _sem(self, key):
        if key not in self.sems:
            self.sems[key] = self.stack.enter_context(self.nc.semaphore(key))
            self.count[key] = 0
        return self.sems[key]

    def begin_phase(self, tag):
        self.ph = tag
        self.pstack = ExitStack()
        self.pstack.__enter__()

    def end_phase(self):
        self.drain_all()
        self.run()
        self.q = {k: [] for k in self.q}
        self.last_w = {}
        self.readers = {}
        self.pstack.__exit__(None, None, None)

    def drain_all(self):
        for qname in self.q:
            waits = []
            for k, v in self.count.items():
                if v > 0 and self.waited.get((qname, k), 0) < v:
                    self.waited[(qname, k)] = v
                    waits.append((self.sems[k], v))
            def emit(e, waits=waits):
                for s, v in waits:
                    e.wait_ge(s, v)
            self.q[qname].append(emit)

    def sbuf(self, name, shape, dtype):
        return self.pstack.enter_context(self.nc.sbuf_tensor(f"s{self.ph}_" + name, list(shape), dtype))

    def psum(self, name, shape, dtype=F32):
        return self.pstack.enter_context(self.nc.psum_tensor(f"p{self.ph}_" + name, list(shape), dtype))

    def collective(self, kind, ins, outs, groups, reads, writes, slot):
        key = "C_" + slot
        self._sem(key)
        deps = self._deps(reads, writes, skip_key=None)
        waits = self._emit_waits("pool", deps)
        self.count[key] += 1
        val = self.count[key]
        sem = self.sems[key]

        def emit(e, waits=waits, sem=sem):
            for s, v in waits:
                e.wait_ge(s, v)
            e.collective_compute(kind, ALU.bypass, replica_groups=groups, ins=ins, outs=outs).then_inc(sem, 1)
        self.q["pool"].append(emit)
        self._commit(reads, writes, (key, val))

    def _deps(self, reads, writes, skip_key=None):
        deps = {}
        def add(ev):
            if ev is None:
                return
            k, v = ev
            if k == skip_key:
                return
            if deps.get(k, 0) < v:
                deps[k] = v
        for r in reads:
            add(self.last_w.get(r))
        for w in writes:
            add(self.last_w.get(w))
            for ev in self.readers.get(w, ()):
                add(ev)
        return deps

    def _commit(self, reads, writes, ev):
        for r in reads:
            self.readers.setdefault(r, []).append(ev)
        for w in writes:
            self.last_w[w] = ev
            self.readers[w] = []

    def _emit_waits(self, qname, deps):
        out = []
        for k, v in deps.items():
            if self.waited.get((qname, k), 0) >= v:
                continue
            self.waited[(qname, k)] = v
            out.append((self.sems[k], v))
        return out

    def op(self, qname, fn, reads=(), writes=(), pe_acc=False):
        key = "E_" + qname
        deps = self._deps(reads, writes, skip_key=(key if (qname == "pe") else None))
        waits = self._emit_waits(qname, deps)
        self.count[key] += 1
        val = self.count[key]
        sem = self.sems[key]
        self.n_ops += 1
        self.n_waits += len(waits)

        def emit(e, fn=fn, waits=waits, sem=sem):
            for s, v in waits:
                e.wait_ge(s, v)
            fn(e).then_inc(sem, 1)
        self.q[qname].append(emit)
        self._commit(reads, writes, (key, val))

    def I(self, qname, method, *args, reads=(), writes=(), **kw):
        self.op(qname, (lambda e, m=method, a=args, k=kw: getattr(e, m)(*a, **k)), reads, writes)

    def dma(self, qname, out, in_, reads=(), writes=(), slot=None, **kw):
        assert slot is not None
        key = "D_" + slot
        self._sem(key)
        deps = self._deps(reads, writes, skip_key=key)
        waits = self._emit_waits(qname, deps)
        self.count[key] += 16
        val = self.count[key]
        sem = self.sems[key]
        self.n_ops += 1
        self.n_waits += len(waits)

        def emit(e, waits=waits, sem=sem, out=out, in_=in_, kw=kw):
            for s, v in waits:
                e.wait_ge(s, v)
            e.dma_start(out=out, in_=in_, **kw).then_inc(sem, 16)
        self.q[qname].append(emit)
        self._commit(reads, writes, (key, val))

    def finish(self, qname, resources):
        deps = self._deps(resources, resources)
        waits = self._emit_waits(qname, deps)

        def emit(e, waits=waits):
            for s, v in waits:
                e.wait_ge(s, v)
        self.q[qname].append(emit)

    def run(self):
        nc = self.nc
        with nc.Block() as block:
            @block.tensor
            def _(e):
                for f in self.q["pe"]:
                    f(e)

            @block.scalar
            def _(e):
                for f in self.q["act"]:
                    f(e)

            @block.vector
            def _(e):
                for f in self.q["dve"]:
                    f(e)

            @block.gpsimd
            def _(e):
                for f in self.q["pool"]:
                    f(e)

            @block.sync
            def _(e):
                for f in self.q["sp"]:
                    f(e)


NEG = -30000.0
EPS = 1e-6
TC = 512


def emit_p1(P, S, d, L, x_chunk, ysrc, piece_done=None, chunks_per_piece=2):
    nc = P.nc
    NT = S // TC
    NKT = S // 128
    d = {"gk": d["gk1"][L], "w_in": d["w_in"][L], "conv_w": d["conv_w"][L], "vecs": d["vecs1"][L],
         "w_r": d["w_r"][L], "w_i": d["w_i"][L], "consts": d["consts1"]}
    if True:
        if True:
            pass
        cst = P.sbuf("cst", [128, 3 * 128 + 4 * 512], BF16)
        P.dma("pool", cst[:], d["consts"], writes=["cst"], slot="cst")
        ones = cst[:, 0:128]
        trineg = cst[:, 128:256]
        ident = cst[:, 256:384]
        masks = [cst[:, 384 + i * 512: 384 + (i + 1) * 512] for i in range(4)]
        negones = P.sbuf("negones", [128, 128], BF16)
        P.op("dve", lambda e: e.tensor_scalar(negones[:], ones, -1.0, None, ALU.mult), reads=["cst"], writes=["negones"])

        gk = P.sbuf("gk", [128, 8], F32)
        P.dma("sp", gk[:], d["gk"], writes=["gk"], slot="gk")
        convw = P.sbuf("convw", [128, 2, 4], F32)
        P.dma("sp", convw[:], d["conv_w"], writes=["convw"], slot="convw")
        vecs = P.sbuf("vecs", [128, 2, 4], F32)
        P.dma("sp", vecs[:], d["vecs"], writes=["vecs"], slot="vecs")
        c1 = P.sbuf("c1", [128, 2], F32)
        for cc in range(2):
            P.op("act", lambda e, cc=cc: e.activation(c1[:, cc:cc + 1], vecs[:, cc, 3:4], AF.Exp, scale=-1.0), reads=["vecs"], writes=["c1"])
        P.op("act", lambda e: e.activation(c1[:], c1[:], AF.Ln, bias=1.0), reads=["c1"], writes=["c1"])
        P.op("dve", lambda e: e.tensor_scalar(c1[:], c1[:], -8.0, None, ALU.mult), reads=["c1"], writes=["c1"])

        bd_f = P.sbuf("bd_f", [128, 4, 128], F32)
        P.op("pool", lambda e: e.memset(bd_f[:], 0.0), writes=["bd_f"])
        for gi, nm in enumerate(("w_r", "w_i")):
            for cc in range(2):
                for hb in range(2):
                    P.dma("sp", bd_f[hb * 64:(hb + 1) * 64, gi * 2 + cc, hb * 64:(hb + 1) * 64], d[nm][cc * 2 + hb],
                          reads=[], writes=["bd_f"], slot="bd_f")
        bd = P.sbuf("bd", [128, 4, 128], BF16)
        P.op("dve", lambda e: e.tensor_copy(bd[:], bd_f[:]), reads=["bd_f"], writes=["bd"])

        wbf = P.sbuf("wbf", [128, 8, 1280], BF16)
        wst = [P.sbuf(f"wst{i}", [128, 1280], F32) for i in range(2)]
        w_in_v = d["w_in"].rearrange("(k p) c -> p k c", p=128)
        for k in range(8):
            b = k % 2
            P.dma("sp", wst[b][:], w_in_v[:, k, :], writes=[f"wst{b}"], slot=f"wst{b}")
            P.op("dve", lambda e, k=k, b=b: e.tensor_scalar(wbf[:, k, :], wst[b][:], gk[:, k:k + 1], None, ALU.mult),
                 reads=[f"wst{b}", "gk"], writes=["wbf"])
        P.op("dve", lambda e: e.tensor_scalar(wbf[:, :, 512:768], wbf[:, :, 512:768], 0.125, None, ALU.mult), reads=["wbf"], writes=["wbf"])

        kT = P.sbuf("kT", [128, 2, S], BF16)
        V = P.sbuf("V", [128, NKT, 256], BF16)
        xt = P.sbuf("xt", [128, 8, TC], F32)
        sq = P.sbuf("sq", [128, 8, TC], BF16)
        hT = P.sbuf("hT", [128, 8, TC], BF16)
        rstd = P.sbuf("rstd", [128, TC], F32)
        xl = P.sbuf("xl", [128, 2, 3 + TC], F32)
        P.op("pool", lambda e: e.memset(xl[:], 0.0), writes=["xl0", "xl1"])
        xc = P.sbuf("xc", [128, TC], F32)
        xcb = P.sbuf("xcb", [128, TC], BF16)
        rr = P.sbuf("rr", [128, TC], F32)
        ii = P.sbuf("ii", [128, TC], F32)
        ss = P.sbuf("ss", [128, TC], F32)
        hb_ = [P.sbuf(f"hbuf{i}", [128, 2, TC], F32) for i in range(2)]
        gt = P.sbuf("gt", [128, TC], F32)
        t1 = P.sbuf("t1", [128, TC], F32)
        yl = P.sbuf("yl", [128, 2, TC], F32)
        ebuf = [P.sbuf(f"e{i}", [128, 2 * TC], F32) for i in range(2)]
        spb = [P.sbuf(f"sp{i}", [128, 2 * TC], BF16) for i in range(2)]
        spd = [P.sbuf(f"spd{i}", [128, 2 * TC], BF16) for i in range(3)]
        wd = [P.sbuf(f"wd{i}", [128, 2 * TC], BF16) for i in range(3)]
        for i in range(3):
            P.I("pool", "memset", spd[i][:], 0.0, writes=[f"spd{i}"])
            P.I("pool", "memset", wd[i][:], 0.0, writes=[f"wd{i}"])
        wb = [P.sbuf(f"w{i}", [128, 2 * TC], BF16) for i in range(2)]
        Rb = [P.sbuf(f"R{i}", [128, 2 * TC], BF16) for i in range(2)]
        ost = [P.sbuf(f"ost{i}", [128, TC], F32) for i in range(2)]
        qT2 = P.sbuf("qT2", [128, 4, TC], BF16)
        nvecs = P.sbuf("nvecs", [128, 2, 4], F32)
        P.I("dve", "tensor_scalar", nvecs[:], vecs[:], -1.0, None, ALU.mult, reads=["vecs"], writes=["nvecs"])
        psA2 = [P.psum(f"psA{i}", [128, 2 * TC]) for i in range(3)]
        psO1 = P.psum("psO", [128, TC])
        pq_ = P.psum("pq", [128, TC])
        pq = [pq_, pq_]
        npp = [0]

        def prep(tc):
            t0 = tc * TC
            qb = (tc % 2) * 2
            P.dma("sp", xt[:], x_chunk(tc), writes=["xt"], slot="xt")
            for k in range(8):
                P.I("pool", "tensor_tensor", sq[:, k, :], xt[:, k, :], xt[:, k, :], ALU.mult, reads=["xt"], writes=["sq"])
                if k % 4 == 3:
                    yield
            for k in range(8):
                P.I("pe", "matmul", pq[0][:], ones, sq[:, k, :], start=(k == 0), stop=(k == 7), reads=["cst", "sq"], writes=["pq0"])
                if k % 4 == 3:
                    yield
            P.I("act", "activation", rstd[:], pq[0][:], AF.Ln, scale=1.0 / 1024, bias=EPS, reads=["pq0"], writes=["rstd"])
            P.I("act", "activation", rstd[:], rstd[:], AF.Exp, scale=-0.5, reads=["rstd"], writes=["rstd"])
            yield
            for k in range(8):
                en = "dve" if k % 2 == 0 else "pool"
                P.I(en, "tensor_tensor", hT[:, k, :], xt[:, k, :], rstd[:], ALU.mult, reads=["xt", "rstd"], writes=[f"hT{k}"])
                if k % 4 == 3:
                    yield
            for cc in range(8):
                pb = npp[0] % 2
                npp[0] += 1
                ps, psn = pq[pb], "pq0"
                for k in range(8):
                    P.I("pe", "matmul", ps[:], wbf[:, k, cc * 128:(cc + 1) * 128], hT[:, k, :], start=(k == 0), stop=(k == 7),
                        reads=["wbf", f"hT{k}"], writes=[psn])
                    if k % 4 == 3:
                        yield
                if cc < 2:
                    P.I("dve", "tensor_copy", xl[:, cc, 3:3 + TC], ps[:], reads=[psn], writes=[f"xl{cc}"])
                elif cc < 4:
                    P.I("dve", "tensor_copy", gt[:], ps[:], reads=[psn], writes=["gt"])
                    yield
                    yield from lru(cc - 2, tc, t0)
                elif cc < 6:
                    P.I("dve", "tensor_copy", qT2[:, qb + cc - 4, :], ps[:], reads=[psn], writes=[f"qT{qb + cc - 4}"])
                else:
                    P.I("dve", "tensor_copy", kT[:, cc - 6, t0:t0 + TC], ps[:], reads=[psn], writes=[f"kT{cc - 6}_{tc}"])
                yield
            for j in range(4):
                kt = tc * 4 + j
                for k in range(8):
                    P.I("pe", "matmul", pq[1][:, 0:256], hT[:, k, j * 128:(j + 1) * 128], wbf[:, k, 1024:1280], start=(k == 0), stop=(k == 7),
                        reads=["wbf", f"hT{k}"], writes=["pq0"])
                    if k % 4 == 3:
                        yield
                P.I("dve", "tensor_copy", V[:, kt, :], pq[1][:, 0:256], reads=["pq0"], writes=[f"V{kt}"])
                yield

        def sigmoid_chain(buf, bn, src_ap, src_names, scale, bias):
            P.I("act", "activation", buf, src_ap, AF.Exp, scale=-scale, bias=bias, reads=src_names, writes=[bn])
            P.I("act", "activation", buf, buf, AF.Ln, bias=1.0, reads=[bn], writes=[bn])
            P.I("act", "activation", buf, buf, AF.Exp, scale=-1.0, reads=[bn], writes=[bn])

        def lru(cc, tc, t0):
            X = f"xl{cc}"
            P.I("dve", "tensor_scalar", xc[:], xl[:, cc, 3:3 + TC], convw[:, cc, 3:4], vecs[:, cc, 0:1], ALU.mult, ALU.add,
                reads=[X, "convw", "vecs"], writes=["xc"])
            yield
            for j in range(3):
                P.I("dve", "scalar_tensor_tensor", xc[:], xl[:, cc, j:j + TC], convw[:, cc, j:j + 1], xc[:], ALU.mult, ALU.add,
                    reads=[X, "convw", "xc"], writes=["xc"])
                yield
            P.I("pool", "tensor_copy", xl[:, cc, 0:3], xl[:, cc, TC:TC + 3], reads=[X], writes=[X])
            P.I("pool", "tensor_copy", xcb[:], xc[:], reads=["xc"], writes=["xcb"])
            yield
            P.I("pe", "matmul", pq[0][:], bd[:, cc, :], xcb[:], start=True, stop=True, reads=["bd", "xcb"], writes=["pq0"])
            yield
            sigmoid_chain(rr[:], "rr", pq[0][:], ["pq0", "nvecs"], 1.0, nvecs[:, cc, 1:2])
            yield
            P.I("pe", "matmul", pq[0][:], bd[:, 2 + cc, :], xcb[:], start=True, stop=True, reads=["bd", "xcb"], writes=["pq0"])
            yield
            sigmoid_chain(ii[:], "ii", pq[0][:], ["pq0", "nvecs"], 1.0, nvecs[:, cc, 2:3])
            yield
            P.I("act", "activation", rr[:], rr[:], AF.Exp, scale=c1[:, cc:cc + 1], reads=["rr", "c1"], writes=["rr"])
            P.I("dve", "scalar_tensor_tensor", ss[:], rr[:], 0.9999999, rr[:], ALU.min, ALU.mult, reads=["rr"], writes=["ss"])
            yield
            P.I("act", "activation", ss[:], ss[:], AF.Ln, scale=-1.0, bias=1.0, reads=["ss"], writes=["ss"])
            P.I("act", "activation", ss[:], ss[:], AF.Exp, scale=0.5, reads=["ss"], writes=["ss"])
            P.I("dve", "tensor_tensor", ii[:], ii[:], xc[:], ALU.mult, reads=["ii", "xc"], writes=["ii"])
            yield
            P.I("dve", "tensor_tensor", ii[:], ii[:], ss[:], ALU.mult, reads=["ii", "ss"], writes=["ii"])
            hcur, hprev = hb_[tc % 2], hb_[(tc + 1) % 2]
            Hc, Hp = f"h{tc % 2}_{cc}", f"h{(tc + 1) % 2}_{cc}"
            yield
            if tc == 0:
                P.I("dve", "tensor_tensor_scan", hcur[:, cc, :], rr[:], ii[:], 0.0, ALU.mult, ALU.add, reads=["rr", "ii"], writes=[Hc])
            else:
                P.I("dve", "tensor_tensor_scan", hcur[:, cc, :], rr[:], ii[:], hprev[:, cc, TC - 1:TC], ALU.mult, ALU.add,
                    reads=["rr", "ii", Hp], writes=[Hc])
            yield
            P.I("pool", "tensor_tensor", t1[:], gt[:], gt[:], ALU.mult, reads=["gt"], writes=["t1"])
            P.I("dve", "tensor_scalar", t1[:], t1[:], 0.044715, 1.0, ALU.mult, ALU.add, reads=["t1"], writes=["t1"])
            yield
            P.I("pool", "tensor_tensor", t1[:], t1[:], gt[:], ALU.mult, reads=["t1", "gt"], writes=["t1"])
            sigmoid_chain(t1[:], "t1", t1[:], ["t1"], 1.5957691216057308, 0.0)
            yield
            P.I("pool", "tensor_tensor", t1[:], t1[:], gt[:], ALU.mult, reads=["t1", "gt"], writes=["t1"])
            P.I("dve", "tensor_tensor", yl[:, cc, :], hcur[:, cc, :], t1[:], ALU.mult, reads=[Hc, "t1"], writes=[f"yl{cc}"])
            P.dma("sp", ysrc(cc * 128, (cc + 1) * 128, t0, TC), yl[:, cc, :], reads=[f"yl{cc}"], writes=[f"ysrc{tc // chunks_per_piece}_yl{cc}"], slot=f"yl{cc}")
            yield

        def attention(tc, nxt, n_units):
            t0 = tc * TC
            qb = (tc % 2) * 2
            nk = tc * 4 + 4
            steps_left = 2 * (nk + 1)
            units_left = n_units if nxt is not None else 0
            An = lambda s: [f"A{s}h0", f"A{s}h1"]
            for hpair in range(2):
                hc = hpair

                def qk(n):
                    kt = nk - 1 - n
                    s = n % 3
                    diag = kt >= tc * 4
                    for hh in range(2):
                        hp = hh * 64
                        P.I("pe", "matmul", psA2[s][:, hh * TC:(hh + 1) * TC], kT[hp:hp + 64, hc, kt * 128:(kt + 1) * 128], qT2[hp:hp + 64, qb + hc, :],
                            start=True, stop=(not diag), reads=[f"kT{hc}_{kt // 4}", f"qT{qb + hc}"], writes=[f"A{s}h{hh}"])
                    if diag:
                        for hh in range(2):
                            P.I("pe", "matmul", psA2[s][:, hh * TC:(hh + 1) * TC], ident, masks[kt - tc * 4], start=False, stop=True,
                                reads=["cst"], writes=[f"A{s}h{hh}"])

                def spbuf(n):
                    return (spd[n], f"spd{n}") if n < 3 else (spb[n % 2], f"sp{n % 2}")

                def cum(n):
                    s, b = n % 3, n % 2
                    spt, spn = spbuf(n)
                    for hh in range(2):
                        P.I("pe", "matmul", psA2[s][:, hh * TC:(hh + 1) * TC], trineg, spt[:, hh * TC:(hh + 1) * TC], start=False, stop=(n == 0),
                            skip_group_check=True, reads=["cst", spn], writes=[f"A{s}h{hh}"])
                    if n > 0:
                        for hh in range(2):
                            P.I("pe", "matmul", psA2[s][:, hh * TC:(hh + 1) * TC], negones[:], Rb[1 - b][:, hh * TC:(hh + 1) * TC], start=False, stop=True,
                                skip_group_check=True, reads=["negones", f"R{1 - b}"], writes=[f"A{s}h{hh}"])
                        P.I("dve", "tensor_tensor", Rb[b][:], Rb[1 - b][:], spt[:], ALU.add, reads=[f"R{1 - b}", spn], writes=[f"R{b}"])
                    else:
                        P.I("dve", "tensor_copy", Rb[b][:], spt[:], reads=[spn], writes=[f"R{b}"])

                def wbuf(n):
                    return (wd[n], f"wd{n}") if n < 3 else (wb[n % 2], f"w{n % 2}")

                def lo_of(n):
                    return (3 - n) * 128 if n < 3 else 0

                def v3(ap, lo):
                    return ap if lo == 0 else ap.rearrange("p (h t) -> p h t", h=2)[:, :, lo:TC]

                def wv(n):
                    kt = nk - 1 - n
                    wt, wn = wbuf(n)
                    for hh in range(2):
                        h = 2 * hpair + hh
                        P.I("pe", "matmul", psO1[hh * 64:(hh + 1) * 64, :], V[:, kt, h * 64:(h + 1) * 64], wt[:, hh * TC:(hh + 1) * TC],
                            start=(n == 0), stop=(n == nk - 1), reads=[f"V{kt}", wn], writes=[f"O{hh}"])

                qk(0)
                qk(1)
                for n in range(nk + 1):
                    if n < nk:
                        s, b = n % 3, n % 2
                        spt, spn = spbuf(n)
                        lo = lo_of(n)
                        P.I("act", "activation", v3(ebuf[b][:], lo), v3(psA2[s][:], lo), AF.Exp, reads=An(s), writes=[f"e{b}"])
                        P.I("act", "activation", v3(spt[:], lo), v3(ebuf[b][:], lo), AF.Ln, bias=1.0, reads=[f"e{b}"], writes=[spn])
                        cum(n)
                    if n >= 1:
                        s1 = (n - 1) % 3
                        wt, wn = wbuf(n - 1)
                        lo1 = lo_of(n - 1)
                        P.I("act", "activation", v3(wt[:], lo1), v3(psA2[s1][:], lo1), AF.Exp, reads=An(s1), writes=[wn])
                        wv(n - 1)
                    if n + 2 < nk:
                        qk(n + 2)
                    if nxt is not None and units_left > 0:
                        take = -(-units_left // steps_left)
                        for _ in range(take):
                            if next(nxt, "done") == "done":
                                units_left = 0
                                break
                            units_left -= 1
                    steps_left -= 1
                os_ = ost[hpair]
                P.I("dve", "tensor_copy", os_[:], psO1[:], reads=["O0", "O1"], writes=[f"ost{hpair}"])
                P.dma("sp", ysrc(256 + hpair * 128, 256 + (hpair + 1) * 128, t0, TC), os_[:], reads=[f"ost{hpair}"], writes=[f"ysrc{tc // chunks_per_piece}_ost{hpair}"], slot=f"ost{hpair}")
            if nxt is not None:
                for _ in nxt:
                    pass

        n_units = sum(1 for _ in prep(0))
        for tc in range(NT):
            attention(tc, prep(tc + 1) if tc + 1 < NT else None, n_units)
            if piece_done is not None and tc % chunks_per_piece == chunks_per_piece - 1:
                j = tc // chunks_per_piece
                piece_done(j, [f"ysrc{j}_yl0", f"ysrc{j}_yl1", f"ysrc{j}_ost0", f"ysrc{j}_ost1"])
        print("P1 ops", P.n_ops, "waits", P.n_waits)


def p1_consts():
    c = np.zeros((128, 3 * 128 + 4 * 512), np.float32)
    c[:, 0:128] = 1.0
    j = np.arange(128)[:, None]
    s = np.arange(128)[None, :]
    c[:, 128:256] = np.where(j >= s, -1.0, 0.0)
    c[:, 256:384] = np.eye(128)
    t = np.arange(512)[None, :]
    for ki in range(4):
        c[:, 384 + ki * 512: 384 + (ki + 1) * 512] = np.where((ki * 128 + j) < t, 0.0, NEG)
    return c


def p1_inputs_T(xTb, layer, hs, W):
    sl = slice(hs * 256, (hs + 1) * 256)
    w_in = W["w_in"][layer]
    cols = np.concatenate([w_in[:, 0:512][:, sl], w_in[:, 512:1024][:, sl], w_in[:, 1024:1536][:, sl],
                           w_in[:, 1536:2048][:, sl], w_in[:, 2048:2560][:, sl]], axis=1)
    def pc(v):
        return np.ascontiguousarray(v[sl].reshape(2, 128).T)
    conv_w = np.ascontiguousarray(W["conv_w"][layer][:, sl].reshape(4, 2, 128).transpose(2, 1, 0))
    vecs = np.stack([pc(W["conv_b"][layer]), pc(W["b_rgate"][layer].reshape(-1)), pc(W["b_igate"][layer].reshape(-1)),
                     pc(W["lru_lambda"][layer])], axis=2)
    return {
        "gk": np.ascontiguousarray(W["mix_norm"][layer].reshape(8, 128).T),
        "w_in": np.ascontiguousarray(cols),
        "conv_w": conv_w,
        "vecs": np.ascontiguousarray(vecs),
        "w_r": np.ascontiguousarray(W["w_rgate"][layer][hs * 4:(hs + 1) * 4]),
        "w_i": np.ascontiguousarray(W["w_igate"][layer][hs * 4:(hs + 1) * 4]),
        "consts": p1_consts(),
    }


CH = 512


def emit_p2(P, T, TB, moe, final, d, L, x_chunk, y_chunks, out_chunk, out_done=None):
    nc = P.nc
    NB = T // TB
    NCH = TB // CH
    NE = 8 if moe else 1
    j = L // 2
    d = {"vec": d["vec2"][L], "w_out": d["w_out"][L], "consts": d["consts2"], "hmask": d["hmask"],
         "wg": d["moe_wg"] if moe else d["dense_wg"], "wu": d["moe_wu"] if moe else d["dense_wu"],
         "wd": d["moe_wd"] if moe else d["dense_wd"], "w_router": d["w_router"]}
    LRU_K = (0, 1, 4, 5)
    SB_K = (2, 3, 6, 7)
    if True:
        if True:
            pass
        cstf = P.sbuf("cstf", [128, 256 + 1024], F32)
        P.dma("sp", cstf[:], d["consts"], writes=["cstf"], slot="cstf")
        cstb = P.sbuf("cstb", [128, 128], BF16)
        P.I("dve", "tensor_copy", cstb[:], cstf[:, 0:128], reads=["cstf"], writes=["cstb"])
        ones = cstb[:, 0:128]
        onesf = cstf[:, 0:128]
        identf = cstf[:, 128:256]
        sel = [cstf[0:8, 256 + e * 128: 256 + (e + 1) * 128] for e in range(8)]
        vec = P.sbuf("vec", [128, 3, 8], F32)
        P.dma("sp", vec[:], d["vec"], writes=["vec"], slot="vec")

        hmask = P.sbuf("hmask", [128, 2], F32)
        P.dma("sp", hmask[:], d["hmask"], writes=["hmask"], slot="vec")
        xres = P.sbuf("xres", [128, 8, TB], F32)
        hT = P.sbuf("hT", [128, 8, TB], BF16)
        arena = P.sbuf("arena", [128, 24576], BF16)
        def wview(b, which):
            base = b * 12288
            if which == "g":
                return arena[:, base:base + 4096].rearrange("p (k f) -> p k f", k=8)
            if which == "u":
                return arena[:, base + 4096:base + 8192].rearrange("p (k f) -> p k f", k=8)
            return arena[:, base + 8192:base + 12288].rearrange("p (j d) -> p j d", j=4)
        wout = arena[:, 0:8192].rearrange("p (k c) -> p k c", k=8)
        yst = P.sbuf("yst", [128, 8, CH], F32)
        yst2 = arena[:, 12288:12288 + 8192].bitcast(F32).rearrange("p (k t) -> p k t", k=8)
        sq = P.sbuf("sq", [128, 8, CH], BF16)
        rs = [P.sbuf(f"rs{i}", [128, CH], F32) for i in range(2)]
        sb_ = [P.sbuf(f"s{i}", [128, CH], BF16) for i in range(2)]
        sg_ = [P.sbuf(f"sg{i}", [128, CH], BF16) for i in range(2)]
        act = [P.sbuf(f"act{i}", [128, 4, CH], BF16) for i in range(2)]
        psG = [P.psum(f"psG{i}", [128, CH]) for i in range(2)]
        psU = [P.psum(f"psU{i}", [128, CH]) for i in range(2)]
        psD = [P.psum(f"psD{i}", [128, CH]) for i in range(4)]
        psS = [psD[2], psD[3]]
        if moe:
            wr = P.sbuf("wr", [128, 8, 8], F32)
            P.dma("sp", wr[:], d["w_router"].rearrange("(k p) e -> p k e", p=128), writes=["wr"], slot="wr")
            for k in range(8):
                P.I("dve", "tensor_scalar", wr[:, k, :], wr[:, k, :], vec[:, 1, k:k + 1], None, ALU.mult, reads=["wr", "vec"], writes=["wr"])
            GT = P.sbuf("GT", [8, TB], F32)
            Gb = P.sbuf("Gb", [128, TB], BF16)
            lg = P.sbuf("lg", [128, 8], F32)
            mx = P.sbuf("mx", [128, 8], F32)
            gsm = P.sbuf("gsm", [128, 4], F32)
            m1 = P.sbuf("m1", [128, 8], F32)
            m2 = P.sbuf("m2", [128, 8], F32)
            rtok = P.sbuf("rtok", [128, 2], F32)

        cnt = {"g": 0, "d": 0}
        wcnt = [0]

        def rms_bcast(src_k, scale_n, ps, out_rs, kr, src_names):
            for k in kr:
                P.I("act", "activation", sq[:, k, :], src_k(k), AF.Square, reads=src_names(k), writes=[f"sq{k}"])
            for n, k in enumerate(kr):
                P.I("pe", "matmul", ps[0][:], ones, sq[:, k, :], start=(n == 0), stop=(n == len(kr) - 1),
                    reads=["cstb", f"sq{k}"], writes=[ps[1]])
            P.I("act", "activation", out_rs[0][:], ps[0][:], AF.Ln, scale=1.0 / scale_n, bias=EPS, reads=[ps[1]], writes=[out_rs[1]])
            P.I("act", "activation", out_rs[0][:], out_rs[0][:], AF.Exp, scale=-0.5, reads=[out_rs[1]], writes=[out_rs[1]])

        for blk in range(NB):
            b0 = blk * TB
            P.dma("pool", wout, d["w_out"].rearrange("(k p) c -> p k c", p=128), writes=["arena0"], slot="arena0")
            for c in range(NCH):
                P.dma("sp", xres[:, :, c * CH:(c + 1) * CH], x_chunk(blk, c), writes=[f"xres{k}_{c}" for k in range(8)], slot=f"xres{c}")
            for c in range(NCH):
                c0 = c * CH
                yA, yB = y_chunks(blk, c)
                P.dma("sp", yst[:], yA, writes=[f"yst{k}" for k in range(8)], slot="yst")
                P.dma("sp", yst2, yB, writes=["arena1"], slot="yst2")
                for k in range(8):
                    P.I("dve", "tensor_scalar", yst[:, k, :], yst[:, k, :], hmask[:, 0:1], None, ALU.mult, reads=[f"yst{k}", "hmask"], writes=[f"yst{k}"])
                    P.I("dve", "scalar_tensor_tensor", yst[:, k, :], yst2[:, k, :], hmask[:, 1:2], yst[:, k, :], ALU.mult, ALU.add,
                        reads=["arena1", f"yst{k}", "hmask"], writes=[f"yst{k}"])
                for g in range(2):
                    kr = list(LRU_K if g == 0 else SB_K)
                    rms_bcast(lambda k: yst[:, k, :], 512.0, (psS[g], f"psD{2 + g}"), (rs[g], f"rs{g}"), kr, lambda k: [f"yst{k}"])
                    for k in kr:
                        P.I("dve", "scalar_tensor_tensor", hT[:, k, c0:c0 + CH], yst[:, k, :], vec[:, 0, k:k + 1], rs[g][:], ALU.mult, ALU.mult,
                            reads=[f"yst{k}", "vec", f"rs{g}"], writes=[f"hT{k}_{c}"])
                for dc in range(8):
                    pb = cnt["d"] % 4
                    cnt["d"] += 1
                    for k in range(8):
                        P.I("pe", "matmul", psD[pb][:], wout[:, k, dc * 128:(dc + 1) * 128], hT[:, k, c0:c0 + CH], start=(k == 0), stop=(k == 7),
                            reads=["arena0", f"hT{k}_{c}"], writes=[f"psD{pb}"])
                    P.I("dve", "tensor_tensor", xres[:, dc, c0:c0 + CH], xres[:, dc, c0:c0 + CH], psD[pb][:], ALU.add,
                        reads=[f"xres{dc}_{c}", f"psD{pb}"], writes=[f"xres{dc}_{c}"])
            for c in range(NCH):
                c0 = c * CH
                g = c % 2
                rms_bcast(lambda k: xres[:, k, c0:c0 + CH], 1024.0, (psS[g], f"psD{2 + g}"), (rs[g], f"rs{g}"), list(range(8)),
                          lambda k: [f"xres{k}_{c}"])
                for k in range(8):
                    P.I("dve", "scalar_tensor_tensor", hT[:, k, c0:c0 + CH], xres[:, k, c0:c0 + CH], vec[:, 1, k:k + 1], rs[g][:], ALU.mult, ALU.mult,
                        reads=[f"xres{k}_{c}", "vec", f"rs{g}"], writes=[f"hT{k}_{c}"])
                if moe:
                    for j in range(4):
                        tt = c0 + j * 128
                        pg = psG[j % 2]
                        pgn = f"psG{j % 2}"
                        for k in range(8):
                            P.I("pe", "matmul", pg[:, 0:8], xres[:, k, tt:tt + 128], wr[:, k, :], start=(k == 0), stop=(k == 7),
                                reads=[f"xres{k}_{c}", "wr"], writes=[pgn])
                        for k in range(8):
                            P.I("pe", "matmul", pg[:, 8:9], sq[:, k, j * 128:(j + 1) * 128], cstb[:, 0:1], start=(k == 0), stop=(k == 7),
                                reads=[f"sq{k}", "cstb"], writes=[pgn], skip_group_check=True)
                        P.I("act", "activation", rtok[:, 0:1], pg[:, 8:9], AF.Sqrt, scale=1.0 / 1024, bias=EPS, reads=[pgn], writes=["rtok"])
                        P.I("dve", "reciprocal", rtok[:, 1:2], rtok[:, 0:1], reads=["rtok"], writes=["rtok"])
                        P.I("dve", "tensor_scalar", lg[:], pg[:, 0:8], rtok[:, 1:2], None, ALU.mult, reads=[pgn, "rtok"], writes=["lg"])
                        P.I("dve", "max", mx[:], lg[:], reads=["lg"], writes=["mx"])
                        P.I("dve", "tensor_tensor", gsm[:, 0:1], mx[:, 0:1], mx[:, 1:2], ALU.subtract, reads=["mx"], writes=["gsm"])
                        P.I("act", "activation", gsm[:, 1:2], gsm[:, 0:1], AF.Sigmoid, reads=["gsm"], writes=["gsm"])
                        P.I("dve", "tensor_scalar", gsm[:, 2:3], gsm[:, 1:2], -1.0, 1.0, ALU.mult, ALU.add, reads=["gsm"], writes=["gsm"])
                        P.I("dve", "tensor_scalar", m1[:], lg[:], mx[:, 0:1], gsm[:, 1:2], ALU.is_equal, ALU.mult, reads=["lg", "mx", "gsm"], writes=["m1"])
                        P.I("dve", "tensor_scalar", m2[:], lg[:], mx[:, 1:2], gsm[:, 2:3], ALU.is_equal, ALU.mult, reads=["lg", "mx", "gsm"], writes=["m2"])
                        P.I("dve", "tensor_tensor", m1[:], m1[:], m2[:], ALU.add, reads=["m1", "m2"], writes=["m1"])
                        pu = psU[j % 2]
                        pun = f"psU{j % 2}"
                        P.I("pe", "transpose", pu[0:8, 0:128], m1[:], identf, reads=["m1", "cstf"], writes=[pun])
                        P.I("dve", "tensor_copy", GT[:, tt:tt + 128], pu[0:8, 0:128], reads=[pun], writes=[f"GT{c}"])
            for e in range(NE):
                if moe:
                    for c in range(NCH):
                        c0 = c * CH
                        pb = cnt["d"] % 4
                        cnt["d"] += 1
                        P.I("pe", "matmul", psD[pb][:], sel[e], GT[:, c0:c0 + CH], start=True, stop=True, reads=["cstf", f"GT{c}"], writes=[f"psD{pb}"])
                        P.I("act", "activation", Gb[:, c0:c0 + CH], psD[pb][:], AF.Copy, reads=[f"psD{pb}"], writes=[f"Gb{c}"])
                for fg in range(6):
                    wb = wcnt[0] % 2
                    wcnt[0] += 1
                    wg_, wu_, wd_ = wview(wb, "g"), wview(wb, "u"), wview(wb, "d")
                    an = f"arena{wb}"
                    P.dma("pool", wg_, d["wg"][e].rearrange("(k p) f -> p k f", p=128)[:, :, fg * 512:(fg + 1) * 512], writes=[an], slot=an)
                    P.dma("pool", wu_, d["wu"][e].rearrange("(k p) f -> p k f", p=128)[:, :, fg * 512:(fg + 1) * 512], writes=[an], slot=an)
                    P.dma("pool", wd_, d["wd"][e][fg * 512:(fg + 1) * 512, :].rearrange("(j p) c -> p j c", p=128), writes=[an], slot=an)
                    for c in range(NCH):
                        c0 = c * CH
                        ab = (cnt["g"] // 4) % 2
                        for fj in range(4):
                            gb = cnt["g"] % 2
                            cnt["g"] += 1
                            for k in range(8):
                                P.I("pe", "matmul", psG[gb][:], wg_[:, k, fj * 128:(fj + 1) * 128], hT[:, k, c0:c0 + CH], start=(k == 0), stop=(k == 7),
                                    reads=[an, f"hT{k}_{c}"], writes=[f"psG{gb}"])
                            for k in range(8):
                                P.I("pe", "matmul", psU[gb][:], wu_[:, k, fj * 128:(fj + 1) * 128], hT[:, k, c0:c0 + CH], start=(k == 0), stop=(k == 7),
                                    reads=[an, f"hT{k}_{c}"], writes=[f"psU{gb}"])
                            P.I("act", "activation", sb_[gb][:], psG[gb][:], AF.Silu, reads=[f"psG{gb}"], writes=[f"s{gb}"])
                            if moe:
                                P.I("dve", "tensor_tensor", sg_[gb][:], sb_[gb][:], Gb[:, c0:c0 + CH], ALU.mult, reads=[f"s{gb}", f"Gb{c}"], writes=[f"sg{gb}"])
                                src, srcn = sg_[gb], f"sg{gb}"
                            else:
                                src, srcn = sb_[gb], f"s{gb}"
                            P.I("dve", "tensor_tensor", act[ab][:, fj, :], src[:], psU[gb][:], ALU.mult, reads=[srcn, f"psU{gb}"], writes=[f"act{ab}_{fj}"])
                        for dc in range(8):
                            pb = cnt["d"] % 4
                            cnt["d"] += 1
                            for fj in range(4):
                                P.I("pe", "matmul", psD[pb][:], wd_[:, fj, dc * 128:(dc + 1) * 128], act[ab][:, fj, :], start=(fj == 0), stop=(fj == 3),
                                    reads=[an, f"act{ab}_{fj}"], writes=[f"psD{pb}"])
                            P.I("dve", "tensor_tensor", xres[:, dc, c0:c0 + CH], xres[:, dc, c0:c0 + CH], psD[pb][:], ALU.add,
                                reads=[f"xres{dc}_{c}", f"psD{pb}"], writes=[f"xres{dc}_{c}"])
            for c in range(NCH):
                c0 = c * CH
                if final:
                    g = c % 2
                    rms_bcast(lambda k: xres[:, k, c0:c0 + CH], 1024.0, (psS[g], f"psD{2 + g}"), (rs[g], f"rs{g}"), list(range(8)),
                              lambda k: [f"xres{k}_{c}"])
                    for k in range(8):
                        P.I("dve", "scalar_tensor_tensor", xres[:, k, c0:c0 + CH], xres[:, k, c0:c0 + CH], vec[:, 2, k:k + 1], rs[g][:], ALU.mult, ALU.mult,
                            reads=[f"xres{k}_{c}", "vec", f"rs{g}"], writes=[f"xres{k}_{c}"])
                P.dma("sp", out_chunk(blk, c), xres[:, :, c0:c0 + CH], reads=[f"xres{k}_{c}" for k in range(8)], writes=[f"p2out{c}"], slot=f"out{c}")
                if out_done is not None:
                    out_done(blk, c, [f"p2out{c}"])
        print("P2 ops", P.n_ops, "waits", P.n_waits)


def p2_consts():
    c = np.zeros((128, 256 + 1024), np.float32)
    c[:, 0:128] = 1.0
    c[:, 128:256] = np.eye(128)
    for e in range(8):
        c[e, 256 + e * 128: 256 + (e + 1) * 128] = 1.0
    return c


def p2_inputs(xT, yT, layer, W, moe):
    def pk(v):
        return np.ascontiguousarray(v.reshape(8, 128).T)
    gy = np.concatenate([W["lru_out_norm"][layer], W["sb_out_norm"][layer]])
    vec = np.stack([pk(gy), pk(W["ffn_norm"][layer]), pk(W["final_norm"])], axis=1)
    j = layer // 2
    r = {"xT": xT, "yT": yT, "vec": np.ascontiguousarray(vec), "w_out": W["w_out"][layer], "consts": p2_consts()}
    if moe:
        r.update(wg=W["moe_w_gate"][j], wu=W["moe_w_up"][j], wd=W["moe_w_down"][j], w_router=W["router_w"][j])
    else:
        r.update(wg=W["dense_w_gate"][j][None], wu=W["dense_w_up"][j][None], wd=W["dense_w_down"][j][None])
    return r


from concourse.bass_utils import run_bass_kernel_spmd

S_FULL = 8192
T_CORE = 4096
TB_P2 = 2048
PAIRS = [[0, 1], [2, 3], [4, 5], [6, 7]]
_CACHE = {}


def build_fused(S=S_FULL, T=T_CORE, TB=TB_P2):
    nc = bass.Bass("TRN2", target_bir_lowering=False)
    def din(name, shape):
        return nc.dram_tensor(name, list(shape), F32, kind="ExternalInput").ap()
    d = {
        "xT": din("xT", [1024, S]), "xT_own": din("xT_own", [1024, T]),
        "gk1": din("gk1", [2, 128, 8]), "w_in": din("w_in", [2, 1024, 1280]), "conv_w": din("conv_w", [2, 128, 2, 4]),
        "vecs1": din("vecs1", [2, 128, 2, 4]), "w_r": din("w_r", [2, 4, 64, 64]), "w_i": din("w_i", [2, 4, 64, 64]),
        "consts1": din("consts1", [128, 3 * 128 + 4 * 512]),
        "vec2": din("vec2", [2, 128, 3, 8]), "w_out": din("w_out", [2, 1024, 1024]), "consts2": din("consts2", [128, 256 + 1024]),
        "hmask": din("hmask", [128, 2]),
        "dense_wg": din("dense_wg", [1, 1024, 3072]), "dense_wu": din("dense_wu", [1, 1024, 3072]), "dense_wd": din("dense_wd", [1, 3072, 1024]),
        "moe_wg": din("moe_wg", [8, 1024, 3072]), "moe_wu": din("moe_wu", [8, 1024, 3072]), "moe_wd": din("moe_wd", [8, 3072, 1024]),
        "w_router": din("w_router", [1024, 8]),
    }
    outT = nc.dram_tensor("outT", [1024, T], F32, kind="ExternalOutput").ap()
    YP = 1024
    XP = 512
    NYP, NXP = S // YP, T // XP
    ysrc = nc.dram_tensor("ysrc", [NYP, 512, YP], F32).ap()
    ydst = nc.dram_tensor("ydst", [NYP, 1024, YP], F32).ap()
    xsrc = nc.dram_tensor("xsrc", [NXP, 1024, XP], F32).ap()
    xdst = nc.dram_tensor("xdst", [NXP, 2048, XP], F32).ap()
    xT_v = d["xT"].rearrange("(k p) t -> p k t", p=128)
    xo_v = d["xT_own"].rearrange("(k p) t -> p k t", p=128)
    oT_v = outT.rearrange("(k p) t -> p k t", p=128)
    NPC = T // TC

    def ysrc_rows(r0, r1, t0, n):
        return ysrc[t0 // YP][r0:r1, t0 % YP:t0 % YP + n]

    def ych(blk, c):
        o = blk * TB + c * CH
        def piece(tok):
            return ydst[tok // YP].rearrange("(k p) t -> p k t", p=128)[:, :, tok % YP:tok % YP + CH]
        return piece(o), piece(T + o)

    def xs_chunk(blk, c):
        o = blk * TB + c * CH
        return xsrc[o // XP].rearrange("(k p) t -> p k t", p=128)

    def xd_chunk(tc):
        return xdst[tc % NPC].rearrange("(r k p) t -> r p k t", r=2, p=128)[tc // NPC]

    with ExitStack() as st:
        P = Prog(nc, st)
        for L in range(2):
            moe = (L % 2 == 1)
            last = (L == 1)
            P.begin_phase(f"a{L}")
            if L == 0:
                xc = lambda tc: xT_v[:, :, tc * TC:(tc + 1) * TC]
            else:
                xc = xd_chunk
            def y_piece(j, deps):
                P.collective("AllGather", [ysrc[j]], [ydst[j]], PAIRS, reads=deps, writes=[f"ydst{j}"], slot="agy")
            emit_p1(P, S, d, L, xc, ysrc_rows, y_piece, YP // TC)
            P.end_phase()
            P.begin_phase(f"c{L}")
            if L == 0:
                xch = lambda blk, c: xo_v[:, :, blk * TB + c * CH: blk * TB + (c + 1) * CH]
                och = xs_chunk
            else:
                xch = xs_chunk
                och = lambda blk, c: oT_v[:, :, blk * TB + c * CH: blk * TB + (c + 1) * CH]
            def x_piece(blk, c, deps):
                j = (blk * TB + c * CH) // XP
                P.collective("AllGather", [xsrc[j]], [xdst[j]], PAIRS, reads=deps, writes=[f"xdst{j}"], slot="agx")
            emit_p2(P, T, TB, moe, last, d, L, xch, ych, och, None if last else x_piece)
            P.end_phase()
    return nc


def fused_inputs(x, W, c):
    b, hs = c // 2, c % 2
    xTb = np.ascontiguousarray(x[b].T)
    p1 = [p1_inputs_T(None, L, hs, W) for L in range(2)]
    perm = np.concatenate([np.arange(0, 256), np.arange(512, 768), np.arange(256, 512), np.arange(768, 1024)])
    def pk(v):
        return np.ascontiguousarray(v.reshape(8, 128).T)
    vec2 = []
    for L in range(2):
        gy = np.concatenate([W["lru_out_norm"][L], W["sb_out_norm"][L]])[perm]
        vec2.append(np.stack([pk(gy), pk(W["ffn_norm"][L]), pk(W["final_norm"])], axis=1))
    hmask = np.zeros((128, 2), np.float32)
    hmask[:, hs] = 1.0
    return {
        "xT": xTb, "xT_own": np.ascontiguousarray(xTb[:, hs * T_CORE:(hs + 1) * T_CORE]),
        "gk1": np.stack([p["gk"] for p in p1]), "w_in": np.stack([p["w_in"] for p in p1]),
        "conv_w": np.stack([p["conv_w"] for p in p1]), "vecs1": np.stack([p["vecs"] for p in p1]),
        "w_r": np.stack([p["w_r"] for p in p1]), "w_i": np.stack([p["w_i"] for p in p1]),
        "consts1": p1_consts(),
        "vec2": np.ascontiguousarray(np.stack(vec2)), "w_out": np.ascontiguousarray(W["w_out"][:, perm, :]),
        "consts2": p2_consts(), "hmask": hmask,
        "dense_wg": W["dense_w_gate"], "dense_wu": W["dense_w_up"], "dense_wd": W["dense_w_down"],
        "moe_wg": W["moe_w_gate"][0], "moe_wu": W["moe_w_up"][0], "moe_wd": W["moe_w_down"][0],
        "w_router": W["router_w"][0],
    }


def kernel(**inputs):
    W = {k: np.asarray(v, dtype=np.float32) for k, v in inputs.items() if k != "x"}
    x = np.asarray(inputs["x"], dtype=np.float32)
    if "nc" not in _CACHE:
        _CACHE["nc"] = build_fused()
    cores = list(range(8))
    ins = [fused_inputs(x, W, c) for c in cores]
    res = run_bass_kernel_spmd(_CACHE["nc"], ins, core_ids=cores).results
    out = np.empty((4, S_FULL, 1024), np.float32)
    for c in cores:
        b, hs = c // 2, c % 2
        out[b, hs * T_CORE:(hs + 1) * T_CORE, :] = res[c]["outT"].T
    return out
```

```python
from contextlib import ExitStack
import numpy as np
import concourse.bass as bass
import concourse.mybir as mybir

F32 = mybir.dt.float32
BF16 = mybir.dt.bfloat16
U32 = mybir.dt.uint32
AF = mybir.ActivationFunctionType
ALU = mybir.AluOpType
AX = mybir.AxisListType

COMPUTE = ("pe", "act", "dve", "pool")


class Prog:
    def __init__(self, nc, stack):
        self.nc = nc
        self.stack = stack
        self.q = {k: [] for k in ("pe", "act", "dve", "pool", "sp")}
        self.eng = {"pe": nc.tensor, "act": nc.scalar, "dve": nc.vector,
                    "pool": nc.gpsimd, "sp": nc.sync}
        self.sems = {}
        self.count = {}
        self.waited = {}
        self.last_w = {}
        self.readers = {}
        self.n_ops = 0
        self.n_waits = 0
        for k in COMPUTE:
            self._sem("E_" + k)

    def _sem(self, key):
        if key not in self.sems:
            self.sems[key] = self.stack.enter_context(self.nc.semaphore(key))
            self.count[key] = 0
        return self.sems[key]

    def begin_phase(self, tag):
        self.ph = tag
        self.pstack = ExitStack()
        self.pstack.__enter__()

    def end_phase(self):
        self.drain_all()
        self.run()
        self.q = {k: [] for k in self.q}
        self.last_w = {}
        self.readers = {}
        self.pstack.__exit__(None, None, None)

    def drain_all(self):
        for qname in self.q:
            waits = []
            for k, v in self.count.items():
                if v > 0 and self.waited.get((qname, k), 0) < v:
                    self.waited[(qname, k)] = v
                    waits.append((self.sems[k], v))
            def emit(e, waits=waits):
                for s, v in waits:
                    e.wait_ge(s, v)
            self.q[qname].append(emit)

    def sbuf(self, name, shape, dtype):
        return self.pstack.enter_context(self.nc.sbuf_tensor(f"s{self.ph}_" + name, list(shape), dtype))

    def psum(self, name, shape, dtype=F32):
        return self.pstack.enter_context(self.nc.psum_tensor(f"p{self.ph}_" + name, list(shape), dtype))

    def collective(self, kind, ins, outs, groups, reads, writes, slot):
        key = "C_" + slot
        self._sem(key)
        deps = self._deps(reads, writes, skip_key=None)
        waits = self._emit_waits("pool", deps)
        self.count[key] += 1
        val = self.count[key]
        sem = self.sems[key]

        def emit(e, waits=waits, sem=sem):
            for s, v in waits:
                e.wait_ge(s, v)
            e.collective_compute(kind, ALU.bypass, replica_groups=groups, ins=ins, outs=outs).then_inc(sem, 1)
        self.q["pool"].append(emit)
        self._commit(reads, writes, (key, val))

    def _deps(self, reads, writes, skip_key=None):
        deps = {}
        def add(ev):
            if ev is None:
                return
            k, v = ev
            if k == skip_key:
                return
            if deps.get(k, 0) < v:
                deps[k] = v
        for r in reads:
            add(self.last_w.get(r))
        for w in writes:
            add(self.last_w.get(w))
            for ev in self.readers.get(w, ()):
                add(ev)
        return deps

    def _commit(self, reads, writes, ev):
        for r in reads:
            self.readers.setdefault(r, []).append(ev)
        for w in writes:
            self.last_w[w] = ev
            self.readers[w] = []

    def _emit_waits(self, qname, deps):
        out = []
        for k, v in deps.items():
            if self.waited.get((qname, k), 0) >= v:
                continue
            self.waited[(qname, k)] = v
            out.append((self.sems[k], v))
        return out

    def op(self, qname, fn, reads=(), writes=(), pe_acc=False):
        key = "E_" + qname
        deps = self._deps(reads, writes, skip_key=(key if (qname == "pe") else None))
        waits = self._emit_waits(qname, deps)
        self.count[key] += 1
        val = self.count[key]
        sem = self.sems[key]
        self.n_ops += 1
        self.n_waits += len(waits)

        def emit(e, fn=fn, waits=waits, sem=sem):
            for s, v in waits:
                e.wait_ge(s, v)
            fn(e).then_inc(sem, 1)
        self.q[qname].append(emit)
        self._commit(reads, writes, (key, val))

    def I(self, qname, method, *args, reads=(), writes=(), **kw):
        self.op(qname, (lambda e, m=method, a=args, k=kw: getattr(e, m)(*a, **k)), reads, writes)

    def dma(self, qname, out, in_, reads=(), writes=(), slot=None, **kw):
        assert slot is not None
        key = "D_" + slot
        self._sem(key)
        deps = self._deps(reads, writes, skip_key=key)
        waits = self._emit_waits(qname, deps)
        self.count[key] += 16
        val = self.count[key]
        sem = self.sems[key]
        self.n_ops += 1
        self.n_waits += len(waits)

        def emit(e, waits=waits, sem=sem, out=out, in_=in_, kw=kw):
            for s, v in waits:
                e.wait_ge(s, v)
            e.dma_start(out=out, in_=in_, **kw).then_inc(sem, 16)
        self.q[qname].append(emit)
        self._commit(reads, writes, (key, val))

    def finish(self, qname, resources):
        deps = self._deps(resources, resources)
        waits = self._emit_waits(qname, deps)

        def emit(e, waits=waits):
            for s, v in waits:
                e.wait_ge(s, v)
        self.q[qname].append(emit)

    def run(self):
        nc = self.nc
        with nc.Block() as block:
            @block.tensor
            def _(e):
                for f in self.q["pe"]:
                    f(e)

            @block.scalar
            def _(e):
                for f in self.q["act"]:
                    f(e)

            @block.vector
            def _(e):
                for f in self.q["dve"]:
                    f(e)

            @block.gpsimd
            def _(e):
                for f in self.q["pool"]:
                    f(e)

            @block.sync
            def _(e):
                for f in self.q["sp"]:
                    f(e)


NEG = -30000.0
EPS = 1e-6
TC = 512


def emit_p1(P, S, d, L, x_chunk, ysrc, piece_done=None, chunks_per_piece=2):
    nc = P.nc
    NT = S // TC
    NKT = S // 128
    d = {"gk": d["gk1"][L], "w_in": d["w_in"][L], "conv_w": d["conv_w"][L], "vecs": d["vecs1"][L],
         "w_r": d["w_r"][L], "w_i": d["w_i"][L], "consts": d["consts1"]}
    if True:
        if True:
            pass
        cst = P.sbuf("cst", [128, 3 * 128 + 4 * 512], BF16)
        P.dma("pool", cst[:], d["consts"], writes=["cst"], slot="cst")
        ones = cst[:, 0:128]
        trineg = cst[:, 128:256]
        ident = cst[:, 256:384]
        masks = [cst[:, 384 + i * 512: 384 + (i + 1) * 512] for i in range(4)]
        negones = P.sbuf("negones", [128, 128], BF16)
        P.op("dve", lambda e: e.tensor_scalar(negones[:], ones, -1.0, None, ALU.mult), reads=["cst"], writes=["negones"])

        gk = P.sbuf("gk", [128, 8], F32)
        P.dma("sp", gk[:], d["gk"], writes=["gk"], slot="gk")
        convw = P.sbuf("convw", [128, 2, 4], F32)
        P.dma("sp", convw[:], d["conv_w"], writes=["convw"], slot="convw")
        vecs = P.sbuf("vecs", [128, 2, 4], F32)
        P.dma("sp", vecs[:], d["vecs"], writes=["vecs"], slot="vecs")
        c1 = P.sbuf("c1", [128, 2], F32)
        for cc in range(2):
            P.op("act", lambda e, cc=cc: e.activation(c1[:, cc:cc + 1], vecs[:, cc, 3:4], AF.Exp, scale=-1.0), reads=["vecs"], writes=["c1"])
        P.op("act", lambda e: e.activation(c1[:], c1[:], AF.Ln, bias=1.0), reads=["c1"], writes=["c1"])
        P.op("dve", lambda e: e.tensor_scalar(c1[:], c1[:], -8.0, None, ALU.mult), reads=["c1"], writes=["c1"])

        bd_f = P.sbuf("bd_f", [128, 4, 128], F32)
        P.op("pool", lambda e: e.memset(bd_f[:], 0.0), writes=["bd_f"])
        for gi, nm in enumerate(("w_r", "w_i")):
            for cc in range(2):
                for hb in range(2):
                    P.dma("sp", bd_f[hb * 64:(hb + 1) * 64, gi * 2 + cc, hb * 64:(hb + 1) * 64], d[nm][cc * 2 + hb],
                          reads=[], writes=["bd_f"], slot="bd_f")
        bd = P.sbuf("bd", [128, 4, 128], BF16)
        P.op("dve", lambda e: e.tensor_copy(bd[:], bd_f[:]), reads=["bd_f"], writes=["bd"])

        wbf = P.sbuf("wbf", [128, 8, 1280], BF16)
        wst = [P.sbuf(f"wst{i}", [128, 1280], F32) for i in range(2)]
        w_in_v = d["w_in"].rearrange("(k p) c -> p k c", p=128)
        for k in range(8):
            b = k % 2
            P.dma("sp", wst[b][:], w_in_v[:, k, :], writes=[f"wst{b}"], slot=f"wst{b}")
            P.op("dve", lambda e, k=k, b=b: e.tensor_scalar(wbf[:, k, :], wst[b][:], gk[:, k:k + 1], None, ALU.mult),
                 reads=[f"wst{b}", "gk"], writes=["wbf"])
        P.op("dve", lambda e: e.tensor_scalar(wbf[:, :, 512:768], wbf[:, :, 512:768], 0.125, None, ALU.mult), reads=["wbf"], writes=["wbf"])

        kT = P.sbuf("kT", [128, 2, S], BF16)
        V = P.sbuf("V", [128, NKT, 256], BF16)
        xt = P.sbuf("xt", [128, 8, TC], F32)
        sq = P.sbuf("sq", [128, 8, TC], BF16)
        hT = P.sbuf("hT", [128, 8, TC], BF16)
        rstd = P.sbuf("rstd", [128, TC], F32)
        xl = P.sbuf("xl", [128, 2, 3 + TC], F32)
        P.op("pool", lambda e: e.memset(xl[:], 0.0), writes=["xl0", "xl1"])
        xc = P.sbuf("xc", [128, TC], F32)
        xcb = P.sbuf("xcb", [128, TC], BF16)
        rr = P.sbuf("rr", [128, TC], F32)
        ii = P.sbuf("ii", [128, TC], F32)
        ss = P.sbuf("ss", [128, TC], F32)
        hb_ = [P.sbuf(f"hbuf{i}", [128, 2, TC], F32) for i in range(2)]
        gt = P.sbuf("gt", [128, TC], F32)
        t1 = P.sbuf("t1", [128, TC], F32)
        yl = P.sbuf("yl", [128, 2, TC], F32)
        ebuf = [P.sbuf(f"e{i}", [128, 2 * TC], F32) for i in range(2)]
        spb = [P.sbuf(f"sp{i}", [128, 2 * TC], BF16) for i in range(2)]
        spd = [P.sbuf(f"spd{i}", [128, 2 * TC], BF16) for i in range(3)]
        wd = [P.sbuf(f"wd{i}", [128, 2 * TC], BF16) for i in range(3)]
        for i in range(3):
            P.I("pool", "memset", spd[i][:], 0.0, writes=[f"spd{i}"])
            P.I("pool", "memset", wd[i][:], 0.0, writes=[f"wd{i}"])
        wb = [P.sbuf(f"w{i}", [128, 2 * TC], BF16) for i in range(2)]
        Rb = [P.sbuf(f"R{i}", [128, 2 * TC], BF16) for i in range(2)]
        ost = [P.sbuf(f"ost{i}", [128, TC], F32) for i in range(2)]
        qT2 = P.sbuf("qT2", [128, 4, TC], BF16)
        nvecs = P.sbuf("nvecs", [128, 2, 4], F32)
        P.I("dve", "tensor_scalar", nvecs[:], vecs[:], -1.0, None, ALU.mult, reads=["vecs"], writes=["nvecs"])
        psA2 = [P.psum(f"psA{i}", [128, 2 * TC]) for i in range(3)]
        psO1 = P.psum("psO", [128, TC])
        pq_ = P.psum("pq", [128, TC])
        pq = [pq_, pq_]
        npp = [0]

        def prep(tc):
            t0 = tc * TC
            qb = (tc % 2) * 2
            if tc == 0:
                P.dma("sp", xt[:], x_chunk(0), writes=["xt"], slot="xt")
            for k in range(8):
                P.I("pool", "tensor_tensor", sq[:, k, :], xt[:, k, :], xt[:, k, :], ALU.mult, reads=["xt"], writes=["sq"])
                if k % 4 == 3:
                    yield
            for k in range(8):
                P.I("pe", "matmul", pq[0][:], ones, sq[:, k, :], start=(k == 0), stop=(k == 7), reads=["cst", "sq"], writes=["pq0"])
                if k % 4 == 3:
                    yield
            P.I("act", "activation", rstd[:], pq[0][:], AF.Ln, scale=1.0 / 1024, bias=EPS, reads=["pq0"], writes=["rstd"])
            P.I("act", "activation", rstd[:], rstd[:], AF.Exp, scale=-0.5, reads=["rstd"], writes=["rstd"])
            yield
            for k in range(8):
                en = "dve" if k % 2 == 0 else "pool"
                P.I(en, "tensor_tensor", hT[:, k, :], xt[:, k, :], rstd[:], ALU.mult, reads=["xt", "rstd"], writes=[f"hT{k}"])
                if k % 4 == 3:
                    yield
            if tc + 1 < NT:
                P.dma("sp", xt[:], x_chunk(tc + 1), writes=["xt"], slot="xt")
            for cc in range(8):
                pb = npp[0] % 2
                npp[0] += 1
                ps, psn = pq[pb], "pq0"
                for k in range(8):
                    P.I("pe", "matmul", ps[:], wbf[:, k, cc * 128:(cc + 1) * 128], hT[:, k, :], start=(k == 0), stop=(k == 7),
                        reads=["wbf", f"hT{k}"], writes=[psn])
                    if k % 4 == 3:
                        yield
                if cc < 2:
                    P.I("dve", "tensor_copy", xl[:, cc, 3:3 + TC], ps[:], reads=[psn], writes=[f"xl{cc}"])
                elif cc < 4:
                    P.I("dve", "tensor_copy", gt[:], ps[:], reads=[psn], writes=["gt"])
                    yield
                    yield from lru(cc - 2, tc, t0)
                elif cc < 6:
                    P.I("dve", "tensor_copy", qT2[:, qb + cc - 4, :], ps[:], reads=[psn], writes=[f"qT{qb + cc - 4}"])
                else:
                    P.I("dve", "tensor_copy", kT[:, cc - 6, t0:t0 + TC], ps[:], reads=[psn], writes=[f"kT{cc - 6}_{tc}"])
                yield
            for j in range(4):
                kt = tc * 4 + j
                for k in range(8):
                    P.I("pe", "matmul", pq[1][:, 0:256], hT[:, k, j * 128:(j + 1) * 128], wbf[:, k, 1024:1280], start=(k == 0), stop=(k == 7),
                        reads=["wbf", f"hT{k}"], writes=["pq0"])
                    if k % 4 == 3:
                        yield
                P.I("dve", "tensor_copy", V[:, kt, :], pq[1][:, 0:256], reads=["pq0"], writes=[f"V{kt}"])
                yield

        def sigmoid_chain(buf, bn, src_ap, src_names, scale, bias):
            P.I("act", "activation", buf, src_ap, AF.Exp, scale=-scale, bias=bias, reads=src_names, writes=[bn])
            P.I("act", "activation", buf, buf, AF.Ln, bias=1.0, reads=[bn], writes=[bn])
            P.I("act", "activation", buf, buf, AF.Exp, scale=-1.0, reads=[bn], writes=[bn])

        def lru(cc, tc, t0):
            X = f"xl{cc}"
            P.I("dve", "tensor_scalar", xc[:], xl[:, cc, 3:3 + TC], convw[:, cc, 3:4], vecs[:, cc, 0:1], ALU.mult, ALU.add,
                reads=[X, "convw", "vecs"], writes=["xc"])
            yield
            for j in range(3):
                P.I("dve", "scalar_tensor_tensor", xc[:], xl[:, cc, j:j + TC], convw[:, cc, j:j + 1], xc[:], ALU.mult, ALU.add,
                    reads=[X, "convw", "xc"], writes=["xc"])
                yield
            P.I("pool", "tensor_copy", xl[:, cc, 0:3], xl[:, cc, TC:TC + 3], reads=[X], writes=[X])
            P.I("pool", "tensor_copy", xcb[:], xc[:], reads=["xc"], writes=["xcb"])
            yield
            P.I("pe", "matmul", pq[0][:], bd[:, cc, :], xcb[:], start=True, stop=True, reads=["bd", "xcb"], writes=["pq0"])
            yield
            sigmoid_chain(rr[:], "rr", pq[0][:], ["pq0", "nvecs"], 1.0, nvecs[:, cc, 1:2])
            yield
            P.I("pe", "matmul", pq[0][:], bd[:, 2 + cc, :], xcb[:], start=True, stop=True, reads=["bd", "xcb"], writes=["pq0"])
            yield
            sigmoid_chain(ii[:], "ii", pq[0][:], ["pq0", "nvecs"], 1.0, nvecs[:, cc, 2:3])
            yield
            P.I("act", "activation", rr[:], rr[:], AF.Exp, scale=c1[:, cc:cc + 1], reads=["rr", "c1"], writes=["rr"])
            P.I("dve", "scalar_tensor_tensor", ss[:], rr[:], 0.9999999, rr[:], ALU.min, ALU.mult, reads=["rr"], writes=["ss"])
            yield
            P.I("act", "activation", ss[:], ss[:], AF.Ln, scale=-1.0, bias=1.0, reads=["ss"], writes=["ss"])
            P.I("act", "activation", ss[:], ss[:], AF.Exp, scale=0.5, reads=["ss"], writes=["ss"])
            P.I("dve", "tensor_tensor", ii[:], ii[:], xc[:], ALU.mult, reads=["ii", "xc"], writes=["ii"])
            yield
            P.I("dve", "tensor_tensor", ii[:], ii[:], ss[:], ALU.mult, reads=["ii", "ss"], writes=["ii"])
            hcur, hprev = hb_[tc % 2], hb_[(tc + 1) % 2]
            Hc, Hp = f"h{tc % 2}_{cc}", f"h{(tc + 1) % 2}_{cc}"
            yield
            if tc == 0:
                P.I("dve", "tensor_tensor_scan", hcur[:, cc, :], rr[:], ii[:], 0.0, ALU.mult, ALU.add, reads=["rr", "ii"], writes=[Hc])
            else:
                P.I("dve", "tensor_tensor_scan", hcur[:, cc, :], rr[:], ii[:], hprev[:, cc, TC - 1:TC], ALU.mult, ALU.add,
                    reads=["rr", "ii", Hp], writes=[Hc])
            yield
            P.I("pool", "tensor_tensor", t1[:], gt[:], gt[:], ALU.mult, reads=["gt"], writes=["t1"])
            P.I("dve", "tensor_scalar", t1[:], t1[:], 0.044715, 1.0, ALU.mult, ALU.add, reads=["t1"], writes=["t1"])
            yield
            P.I("pool", "tensor_tensor", t1[:], t1[:], gt[:], ALU.mult, reads=["t1", "gt"], writes=["t1"])
            sigmoid_chain(t1[:], "t1", t1[:], ["t1"], 1.5957691216057308, 0.0)
            yield
            P.I("pool", "tensor_tensor", t1[:], t1[:], gt[:], ALU.mult, reads=["t1", "gt"], writes=["t1"])
            P.I("dve", "tensor_tensor", yl[:, cc, :], hcur[:, cc, :], t1[:], ALU.mult, reads=[Hc, "t1"], writes=[f"yl{cc}"])
            P.dma("sp", ysrc(cc * 128, (cc + 1) * 128, t0, TC), yl[:, cc, :], reads=[f"yl{cc}"], writes=[f"ysrc{tc // chunks_per_piece}_yl{cc}"], slot=f"yl{cc}")
            yield

        def attention(tc, nxt, n_units):
            t0 = tc * TC
            qb = (tc % 2) * 2
            nk = tc * 4 + 4
            steps_left = 2 * (nk + 1)
            units_left = n_units if nxt is not None else 0
            An = lambda s: [f"A{s}h0", f"A{s}h1"]
            for hpair in range(2):
                hc = hpair

                def qk(n):
                    kt = nk - 1 - n
                    s = n % 3
                    diag = kt >= tc * 4
                    for hh in range(2):
                        hp = hh * 64
                        P.I("pe", "matmul", psA2[s][:, hh * TC:(hh + 1) * TC], kT[hp:hp + 64, hc, kt * 128:(kt + 1) * 128], qT2[hp:hp + 64, qb + hc, :],
                            start=True, stop=(not diag), reads=[f"kT{hc}_{kt // 4}", f"qT{qb + hc}"], writes=[f"A{s}h{hh}"])
                    if diag:
                        for hh in range(2):
                            P.I("pe", "matmul", psA2[s][:, hh * TC:(hh + 1) * TC], ident, masks[kt - tc * 4], start=False, stop=True,
                                reads=["cst"], writes=[f"A{s}h{hh}"])

                def spbuf(n):
                    return (spd[n], f"spd{n}") if n < 3 else (spb[n % 2], f"sp{n % 2}")

                def cum(n):
                    s, b = n % 3, n % 2
                    spt, spn = spbuf(n)
                    for hh in range(2):
                        P.I("pe", "matmul", psA2[s][:, hh * TC:(hh + 1) * TC], trineg, spt[:, hh * TC:(hh + 1) * TC], start=False, stop=(n == 0),
                            skip_group_check=True, reads=["cst", spn], writes=[f"A{s}h{hh}"])
                    if n > 0:
                        for hh in range(2):
                            P.I("pe", "matmul", psA2[s][:, hh * TC:(hh + 1) * TC], negones[:], Rb[1 - b][:, hh * TC:(hh + 1) * TC], start=False, stop=True,
                                skip_group_check=True, reads=["negones", f"R{1 - b}"], writes=[f"A{s}h{hh}"])
                        P.I("dve", "tensor_tensor", Rb[b][:], Rb[1 - b][:], spt[:], ALU.add, reads=[f"R{1 - b}", spn], writes=[f"R{b}"])
                    else:
                        P.I("dve", "tensor_copy", Rb[b][:], spt[:], reads=[spn], writes=[f"R{b}"])

                def wbuf(n):
                    return (wd[n], f"wd{n}") if n < 3 else (wb[n % 2], f"w{n % 2}")

                def lo_of(n):
                    return (3 - n) * 128 if n < 3 else 0

                def v3(ap, lo):
                    return ap if lo == 0 else ap.rearrange("p (h t) -> p h t", h=2)[:, :, lo:TC]

                def wv(n):
                    kt = nk - 1 - n
                    wt, wn = wbuf(n)
                    for hh in range(2):
                        h = 2 * hpair + hh
                        P.I("pe", "matmul", psO1[hh * 64:(hh + 1) * 64, :], V[:, kt, h * 64:(h + 1) * 64], wt[:, hh * TC:(hh + 1) * TC],
                            start=(n == 0), stop=(n == nk - 1), reads=[f"V{kt}", wn], writes=[f"O{hh}"])

                qk(0)
                qk(1)
                for n in range(nk + 1):
                    if n < nk:
                        s, b = n % 3, n % 2
                        spt, spn = spbuf(n)
                        lo = lo_of(n)
                        P.I("act", "activation", v3(ebuf[b][:], lo), v3(psA2[s][:], lo), AF.Exp, reads=An(s), writes=[f"e{b}"])
                        P.I("act", "activation", v3(spt[:], lo), v3(ebuf[b][:], lo), AF.Ln, bias=1.0, reads=[f"e{b}"], writes=[spn])
                        cum(n)
                    if n >= 1:
                        s1 = (n - 1) % 3
                        wt, wn = wbuf(n - 1)
                        lo1 = lo_of(n - 1)
                        P.I("act", "activation", v3(wt[:], lo1), v3(psA2[s1][:], lo1), AF.Exp, reads=An(s1), writes=[wn])
                        wv(n - 1)
                    if n + 2 < nk:
                        qk(n + 2)
                    if nxt is not None and units_left > 0:
                        take = -(-units_left // steps_left)
                        for _ in range(take):
                            if next(nxt, "done") == "done":
                                units_left = 0
                                break
                            units_left -= 1
                    steps_left -= 1
                os_ = ost[hpair]
                P.I("dve", "tensor_copy", os_[:], psO1[:], reads=["O0", "O1"], writes=[f"ost{hpair}"])
                P.dma("sp", ysrc(256 + hpair * 128, 256 + (hpair + 1) * 128, t0, TC), os_[:], reads=[f"ost{hpair}"], writes=[f"ysrc{tc // chunks_per_piece}_ost{hpair}"], slot=f"ost{hpair}")
            if nxt is not None:
                for _ in nxt:
                    pass

        n_units = sum(1 for _ in prep(0))
        for tc in range(NT):
            attention(tc, prep(tc + 1) if tc + 1 < NT else None, n_units)
            if piece_done is not None and tc % chunks_per_piece == chunks_per_piece - 1:
                j = tc // chunks_per_piece
                piece_done(j, [f"ysrc{j}_yl0", f"ysrc{j}_yl1", f"ysrc{j}_ost0", f"ysrc{j}_ost1"])
        print("P1 ops", P.n_ops, "waits", P.n_waits)


def p1_consts():
    c = np.zeros((128, 3 * 128 + 4 * 512), np.float32)
    c[:, 0:128] = 1.0
    j = np.arange(128)[:, None]
    s = np.arange(128)[None, :]
    c[:, 128:256] = np.where(j >= s, -1.0, 0.0)
    c[:, 256:384] = np.eye(128)
    t = np.arange(512)[None, :]
    for ki in range(4):
        c[:, 384 + ki * 512: 384 + (ki + 1) * 512] = np.where((ki * 128 + j) < t, 0.0, NEG)
    return c


def p1_inputs_T(xTb, layer, hs, W):
    sl = slice(hs * 256, (hs + 1) * 256)
    w_in = W["w_in"][layer]
    cols = np.concatenate([w_in[:, 0:512][:, sl], w_in[:, 512:1024][:, sl], w_in[:, 1024:1536][:, sl],
                           w_in[:, 1536:2048][:, sl], w_in[:, 2048:2560][:, sl]], axis=1)
    def pc(v):
        return np.ascontiguousarray(v[sl].reshape(2, 128).T)
    conv_w = np.ascontiguousarray(W["conv_w"][layer][:, sl].reshape(4, 2, 128).transpose(2, 1, 0))
    vecs = np.stack([pc(W["conv_b"][layer]), pc(W["b_rgate"][layer].reshape(-1)), pc(W["b_igate"][layer].reshape(-1)),
                     pc(W["lru_lambda"][layer])], axis=2)
    return {
        "gk": np.ascontiguousarray(W["mix_norm"][layer].reshape(8, 128).T),
        "w_in": np.ascontiguousarray(cols),
        "conv_w": conv_w,
        "vecs": np.ascontiguousarray(vecs),
        "w_r": np.ascontiguousarray(W["w_rgate"][layer][hs * 4:(hs + 1) * 4]),
        "w_i": np.ascontiguousarray(W["w_igate"][layer][hs * 4:(hs + 1) * 4]),
        "consts": p1_consts(),
    }


CH = 512


def emit_p2(P, T, TB, moe, final, d, L, x_chunk, y_chunks, out_chunk, out_done=None):
    nc = P.nc
    NB = T // TB
    NCH = TB // CH
    NE = 8 if moe else 1
    j = L // 2
    d = {"vec": d["vec2"][L], "w_out": d["w_out"][L], "consts": d["consts2"], "hmask": d["hmask"],
         "wg": d["moe_wg"] if moe else d["dense_wg"], "wu": d["moe_wu"] if moe else d["dense_wu"],
         "wd": d["moe_wd"] if moe else d["dense_wd"], "w_router": d["w_router"]}
    LRU_K = (0, 1, 4, 5)
    SB_K = (2, 3, 6, 7)
    if True:
        if True:
            pass
        cstf = P.sbuf("cstf", [128, 256 + 1024], F32)
        P.dma("sp", cstf[:], d["consts"], writes=["cstf"], slot="cstf")
        cstb = P.sbuf("cstb", [128, 128], BF16)
        P.I("dve", "tensor_copy", cstb[:], cstf[:, 0:128], reads=["cstf"], writes=["cstb"])
        ones = cstb[:, 0:128]
        onesf = cstf[:, 0:128]
        identf = cstf[:, 128:256]
        sel = [cstf[0:8, 256 + e * 128: 256 + (e + 1) * 128] for e in range(8)]
        vec = P.sbuf("vec", [128, 3, 8], F32)
        P.dma("sp", vec[:], d["vec"], writes=["vec"], slot="vec")

        hmask = P.sbuf("hmask", [128, 2], F32)
        P.dma("sp", hmask[:], d["hmask"], writes=["hmask"], slot="vec")
        xres = P.sbuf("xres", [128, 8, TB], F32)
        hT = P.sbuf("hT", [128, 8, TB], BF16)
        arena = P.sbuf("arena", [128, 24576], BF16)
        def wview(b, which):
            base = b * 12288
            if which == "g":
                return arena[:, base:base + 4096].rearrange("p (k f) -> p k f", k=8)
            if which == "u":
                return arena[:, base + 4096:base + 8192].rearrange("p (k f) -> p k f", k=8)
            return arena[:, base + 8192:base + 12288].rearrange("p (j d) -> p j d", j=4)
        wout = arena[:, 0:8192].rearrange("p (k c) -> p k c", k=8)
        yst = P.sbuf("yst", [128, 8, CH], F32)
        yst2 = arena[:, 12288:12288 + 8192].bitcast(F32).rearrange("p (k t) -> p k t", k=8)
        sq = P.sbuf("sq", [128, 8, CH], BF16)
        rs = [P.sbuf(f"rs{i}", [128, CH], F32) for i in range(2)]
        sb_ = [P.sbuf(f"s{i}", [128, CH], BF16) for i in range(2)]
        sg_ = [P.sbuf(f"sg{i}", [128, CH], BF16) for i in range(2)]
        act = [P.sbuf(f"act{i}", [128, 4, CH], BF16) for i in range(2)]
        psG = [P.psum(f"psG{i}", [128, CH]) for i in range(2)]
        psU = [P.psum(f"psU{i}", [128, CH]) for i in range(2)]
        psD = [P.psum(f"psD{i}", [128, CH]) for i in range(4)]
        psS = [psD[2], psD[3]]
        if moe:
            wr = P.sbuf("wr", [128, 8, 8], F32)
            P.dma("sp", wr[:], d["w_router"].rearrange("(k p) e -> p k e", p=128), writes=["wr"], slot="wr")
            for k in range(8):
                P.I("dve", "tensor_scalar", wr[:, k, :], wr[:, k, :], vec[:, 1, k:k + 1], None, ALU.mult, reads=["wr", "vec"], writes=["wr"])
            GT = P.sbuf("GT", [8, TB], F32)
            Gb = P.sbuf("Gb", [128, TB], BF16)
            lg = P.sbuf("lg", [128, 8], F32)
            mx = P.sbuf("mx", [128, 8], F32)
            gsm = P.sbuf("gsm", [128, 4], F32)
            m1 = P.sbuf("m1", [128, 8], F32)
            m2 = P.sbuf("m2", [128, 8], F32)
            rtok = P.sbuf("rtok", [128, 2], F32)

        cnt = {"g": 0, "d": 0}
        wcnt = [0]

        def rms_bcast(src_k, scale_n, ps, out_rs, kr, src_names):
            for k in kr:
                P.I("act", "activation", sq[:, k, :], src_k(k), AF.Square, reads=src_names(k), writes=[f"sq{k}"])
            for n, k in enumerate(kr):
                P.I("pe", "matmul", ps[0][:], ones, sq[:, k, :], start=(n == 0), stop=(n == len(kr) - 1),
                    reads=["cstb", f"sq{k}"], writes=[ps[1]])
            P.I("act", "activation", out_rs[0][:], ps[0][:], AF.Ln, scale=1.0 / scale_n, bias=EPS, reads=[ps[1]], writes=[out_rs[1]])
            P.I("act", "activation", out_rs[0][:], out_rs[0][:], AF.Exp, scale=-0.5, reads=[out_rs[1]], writes=[out_rs[1]])

        for blk in range(NB):
            b0 = blk * TB
            P.dma("pool", wout, d["w_out"].rearrange("(k p) c -> p k c", p=128), writes=["arena0"], slot="arena0")
            for c in range(NCH):
                P.dma("sp", xres[:, :, c * CH:(c + 1) * CH], x_chunk(blk, c), writes=[f"xres{k}_{c}" for k in range(8)], slot=f"xres{c}")
            for c in range(NCH):
                c0 = c * CH
                yA, yB = y_chunks(blk, c)
                P.dma("sp", yst[:], yA, writes=[f"yst{k}" for k in range(8)], slot="yst")
                P.dma("sp", yst2, yB, writes=["arena1"], slot="yst2")
                for k in range(8):
                    P.I("dve", "tensor_scalar", yst[:, k, :], yst[:, k, :], hmask[:, 0:1], None, ALU.mult, reads=[f"yst{k}", "hmask"], writes=[f"yst{k}"])
                    P.I("dve", "scalar_tensor_tensor", yst[:, k, :], yst2[:, k, :], hmask[:, 1:2], yst[:, k, :], ALU.mult, ALU.add,
                        reads=["arena1", f"yst{k}", "hmask"], writes=[f"yst{k}"])
                for g in range(2):
                    kr = list(LRU_K if g == 0 else SB_K)
                    rms_bcast(lambda k: yst[:, k, :], 512.0, (psS[g], f"psD{2 + g}"), (rs[g], f"rs{g}"), kr, lambda k: [f"yst{k}"])
                    for k in kr:
                        P.I("dve", "scalar_tensor_tensor", hT[:, k, c0:c0 + CH], yst[:, k, :], vec[:, 0, k:k + 1], rs[g][:], ALU.mult, ALU.mult,
                            reads=[f"yst{k}", "vec", f"rs{g}"], writes=[f"hT{k}_{c}"])
                for dc in range(8):
                    pb = cnt["d"] % 4
                    cnt["d"] += 1
                    for k in range(8):
                        P.I("pe", "matmul", psD[pb][:], wout[:, k, dc * 128:(dc + 1) * 128], hT[:, k, c0:c0 + CH], start=(k == 0), stop=(k == 7),
                            reads=["arena0", f"hT{k}_{c}"], writes=[f"psD{pb}"])
                    P.I("dve", "tensor_tensor", xres[:, dc, c0:c0 + CH], xres[:, dc, c0:c0 + CH], psD[pb][:], ALU.add,
                        reads=[f"xres{dc}_{c}", f"psD{pb}"], writes=[f"xres{dc}_{c}"])
            for c in range(NCH):
                c0 = c * CH
                g = c % 2
                rms_bcast(lambda k: xres[:, k, c0:c0 + CH], 1024.0, (psS[g], f"psD{2 + g}"), (rs[g], f"rs{g}"), list(range(8)),
                          lambda k: [f"xres{k}_{c}"])
                for k in range(8):
                    P.I("dve", "scalar_tensor_tensor", hT[:, k, c0:c0 + CH], xres[:, k, c0:c0 + CH], vec[:, 1, k:k + 1], rs[g][:], ALU.mult, ALU.mult,
                        reads=[f"xres{k}_{c}", "vec", f"rs{g}"], writes=[f"hT{k}_{c}"])
                if moe:
                    for j in range(4):
                        tt = c0 + j * 128
                        pg = psG[j % 2]
                        pgn = f"psG{j % 2}"
                        for k in range(8):
                            P.I("pe", "matmul", pg[:, 0:8], xres[:, k, tt:tt + 128], wr[:, k, :], start=(k == 0), stop=(k == 7),
                                reads=[f"xres{k}_{c}", "wr"], writes=[pgn])
                        for k in range(8):
                            P.I("pe", "matmul", pg[:, 8:9], sq[:, k, j * 128:(j + 1) * 128], cstb[:, 0:1], start=(k == 0), stop=(k == 7),
                                reads=[f"sq{k}", "cstb"], writes=[pgn], skip_group_check=True)
                        P.I("act", "activation", rtok[:, 0:1], pg[:, 8:9], AF.Sqrt, scale=1.0 / 1024, bias=EPS, reads=[pgn], writes=["rtok"])
                        P.I("dve", "reciprocal", rtok[:, 1:2], rtok[:, 0:1], reads=["rtok"], writes=["rtok"])
                        P.I("dve", "tensor_scalar", lg[:], pg[:, 0:8], rtok[:, 1:2], None, ALU.mult, reads=[pgn, "rtok"], writes=["lg"])
                        P.I("dve", "max", mx[:], lg[:], reads=["lg"], writes=["mx"])
                        P.I("dve", "tensor_tensor", gsm[:, 0:1], mx[:, 0:1], mx[:, 1:2], ALU.subtract, reads=["mx"], writes=["gsm"])
                        P.I("act", "activation", gsm[:, 1:2], gsm[:, 0:1], AF.Sigmoid, reads=["gsm"], writes=["gsm"])
                        P.I("dve", "tensor_scalar", gsm[:, 2:3], gsm[:, 1:2], -1.0, 1.0, ALU.mult, ALU.add, reads=["gsm"], writes=["gsm"])
                        P.I("dve", "tensor_scalar", m1[:], lg[:], mx[:, 0:1], gsm[:, 1:2], ALU.is_equal, ALU.mult, reads=["lg", "mx", "gsm"], writes=["m1"])
                        P.I("dve", "tensor_scalar", m2[:], lg[:], mx[:, 1:2], gsm[:, 2:3], ALU.is_equal, ALU.mult, reads=["lg", "mx", "gsm"], writes=["m2"])
                        P.I("dve", "tensor_tensor", m1[:], m1[:], m2[:], ALU.add, reads=["m1", "m2"], writes=["m1"])
                        pu = psU[j % 2]
                        pun = f"psU{j % 2}"
                        P.I("pe", "transpose", pu[0:8, 0:128], m1[:], identf, reads=["m1", "cstf"], writes=[pun])
                        P.I("dve", "tensor_copy", GT[:, tt:tt + 128], pu[0:8, 0:128], reads=[pun], writes=[f"GT{c}"])
            for e in range(NE):
                if moe:
                    for c in range(NCH):
                        c0 = c * CH
                        pb = cnt["d"] % 4
                        cnt["d"] += 1
                        P.I("pe", "matmul", psD[pb][:], sel[e], GT[:, c0:c0 + CH], start=True, stop=True, reads=["cstf", f"GT{c}"], writes=[f"psD{pb}"])
                        P.I("act", "activation", Gb[:, c0:c0 + CH], psD[pb][:], AF.Copy, reads=[f"psD{pb}"], writes=[f"Gb{c}"])
                for fg in range(6):
                    wb = wcnt[0] % 2
                    wcnt[0] += 1
                    wg_, wu_, wd_ = wview(wb, "g"), wview(wb, "u"), wview(wb, "d")
                    an = f"arena{wb}"
                    P.dma("pool", wg_, d["wg"][e].rearrange("(k p) f -> p k f", p=128)[:, :, fg * 512:(fg + 1) * 512], writes=[an], slot=an)
                    P.dma("pool", wu_, d["wu"][e].rearrange("(k p) f -> p k f", p=128)[:, :, fg * 512:(fg + 1) * 512], writes=[an], slot=an)
                    P.dma("pool", wd_, d["wd"][e][fg * 512:(fg + 1) * 512, :].rearrange("(j p) c -> p j c", p=128), writes=[an], slot=an)
                    for c in range(NCH):
                        c0 = c * CH
                        ab = (cnt["g"] // 4) % 2
                        for fj in range(4):
                            gb = cnt["g"] % 2
                            cnt["g"] += 1
                            for k in range(8):
                                P.I("pe", "matmul", psG[gb][:], wg_[:, k, fj * 128:(fj + 1) * 128], hT[:, k, c0:c0 + CH], start=(k == 0), stop=(k == 7),
                                    reads=[an, f"hT{k}_{c}"], writes=[f"psG{gb}"])
                            for k in range(8):
                                P.I("pe", "matmul", psU[gb][:], wu_[:, k, fj * 128:(fj + 1) * 128], hT[:, k, c0:c0 + CH], start=(k == 0), stop=(k == 7),
                                    reads=[an, f"hT{k}_{c}"], writes=[f"psU{gb}"])
                            P.I("act", "activation", sb_[gb][:], psG[gb][:], AF.Silu, reads=[f"psG{gb}"], writes=[f"s{gb}"])
                            if moe:
                                P.I("dve", "tensor_tensor", sg_[gb][:], sb_[gb][:], Gb[:, c0:c0 + CH], ALU.mult, reads=[f"s{gb}", f"Gb{c}"], writes=[f"sg{gb}"])
                                src, srcn = sg_[gb], f"sg{gb}"
                            else:
                                src, srcn = sb_[gb], f"s{gb}"
                            P.I("dve", "tensor_tensor", act[ab][:, fj, :], src[:], psU[gb][:], ALU.mult, reads=[srcn, f"psU{gb}"], writes=[f"act{ab}_{fj}"])
                        for dc in range(8):
                            pb = cnt["d"] % 4
                            cnt["d"] += 1
                            for fj in range(4):
                                P.I("pe", "matmul", psD[pb][:], wd_[:, fj, dc * 128:(dc + 1) * 128], act[ab][:, fj, :], start=(fj == 0), stop=(fj == 3),
                                    reads=[an, f"act{ab}_{fj}"], writes=[f"psD{pb}"])
                            P.I("dve", "tensor_tensor", xres[:, dc, c0:c0 + CH], xres[:, dc, c0:c0 + CH], psD[pb][:], ALU.add,
                                reads=[f"xres{dc}_{c}", f"psD{pb}"], writes=[f"xres{dc}_{c}"])
            for c in range(NCH):
                c0 = c * CH
                if final:
                    g = c % 2
                    rms_bcast(lambda k: xres[:, k, c0:c0 + CH], 1024.0, (psS[g], f"psD{2 + g}"), (rs[g], f"rs{g}"), list(range(8)),
                              lambda k: [f"xres{k}_{c}"])
                    for k in range(8):
                        P.I("dve", "scalar_tensor_tensor", xres[:, k, c0:c0 + CH], xres[:, k, c0:c0 + CH], vec[:, 2, k:k + 1], rs[g][:], ALU.mult, ALU.mult,
                            reads=[f"xres{k}_{c}", "vec", f"rs{g}"], writes=[f"xres{k}_{c}"])
                P.dma("sp", out_chunk(blk, c), xres[:, :, c0:c0 + CH], reads=[f"xres{k}_{c}" for k in range(8)], writes=[f"p2out{c}"], slot=f"out{c}")
                if out_done is not None:
                    out_done(blk, c, [f"p2out{c}"])
        print("P2 ops", P.n_ops, "waits", P.n_waits)


def p2_consts():
    c = np.zeros((128, 256 + 1024), np.float32)
    c[:, 0:128] = 1.0
    c[:, 128:256] = np.eye(128)
    for e in range(8):
        c[e, 256 + e * 128: 256 + (e + 1) * 128] = 1.0
    return c


def p2_inputs(xT, yT, layer, W, moe):
    def pk(v):
        return np.ascontiguousarray(v.reshape(8, 128).T)
    gy = np.concatenate([W["lru_out_norm"][layer], W["sb_out_norm"][layer]])
    vec = np.stack([pk(gy), pk(W["ffn_norm"][layer]), pk(W["final_norm"])], axis=1)
    j = layer // 2
    r = {"xT": xT, "yT": yT, "vec": np.ascontiguousarray(vec), "w_out": W["w_out"][layer], "consts": p2_consts()}
    if moe:
        r.update(wg=W["moe_w_gate"][j], wu=W["moe_w_up"][j], wd=W["moe_w_down"][j], w_router=W["router_w"][j])
    else:
        r.update(wg=W["dense_w_gate"][j][None], wu=W["dense_w_up"][j][None], wd=W["dense_w_down"][j][None])
    return r


from concourse.bass_utils import run_bass_kernel_spmd

S_FULL = 8192
T_CORE = 4096
TB_P2 = 2048
PAIRS = [[0, 1], [2, 3], [4, 5], [6, 7]]
_CACHE = {}


def build_fused(S=S_FULL, T=T_CORE, TB=TB_P2):
    nc = bass.Bass("TRN2", target_bir_lowering=False)
    def din(name, shape):
        return nc.dram_tensor(name, list(shape), F32, kind="ExternalInput").ap()
    d = {
        "xT": din("xT", [1024, S]), "xT_own": din("xT_own", [1024, T]),
        "gk1": din("gk1", [2, 128, 8]), "w_in": din("w_in", [2, 1024, 1280]), "conv_w": din("conv_w", [2, 128, 2, 4]),
        "vecs1": din("vecs1", [2, 128, 2, 4]), "w_r": din("w_r", [2, 4, 64, 64]), "w_i": din("w_i", [2, 4, 64, 64]),
        "consts1": din("consts1", [128, 3 * 128 + 4 * 512]),
        "vec2": din("vec2", [2, 128, 3, 8]), "w_out": din("w_out", [2, 1024, 1024]), "consts2": din("consts2", [128, 256 + 1024]),
        "hmask": din("hmask", [128, 2]),
        "dense_wg": din("dense_wg", [1, 1024, 3072]), "dense_wu": din("dense_wu", [1, 1024, 3072]), "dense_wd": din("dense_wd", [1, 3072, 1024]),
        "moe_wg": din("moe_wg", [8, 1024, 3072]), "moe_wu": din("moe_wu", [8, 1024, 3072]), "moe_wd": din("moe_wd", [8, 3072, 1024]),
        "w_router": din("w_router", [1024, 8]),
    }
    outT = nc.dram_tensor("outT", [1024, T], F32, kind="ExternalOutput").ap()
    YP = 1024
    XP = 512
    NYP, NXP = S // YP, T // XP
    ysrc = nc.dram_tensor("ysrc", [NYP, 512, YP], F32).ap()
    ydst = nc.dram_tensor("ydst", [NYP, 1024, YP], F32).ap()
    xsrc = nc.dram_tensor("xsrc", [NXP, 1024, XP], F32).ap()
    xdst = nc.dram_tensor("xdst", [NXP, 2048, XP], F32).ap()
    xT_v = d["xT"].rearrange("(k p) t -> p k t", p=128)
    xo_v = d["xT_own"].rearrange("(k p) t -> p k t", p=128)
    oT_v = outT.rearrange("(k p) t -> p k t", p=128)
    NPC = T // TC

    def ysrc_rows(r0, r1, t0, n):
        return ysrc[t0 // YP][r0:r1, t0 % YP:t0 % YP + n]

    def ych(blk, c):
        o = blk * TB + c * CH
        def piece(tok):
            return ydst[tok // YP].rearrange("(k p) t -> p k t", p=128)[:, :, tok % YP:tok % YP + CH]
        return piece(o), piece(T + o)

    def xs_chunk(blk, c):
        o = blk * TB + c * CH
        return xsrc[o // XP].rearrange("(k p) t -> p k t", p=128)

    def xd_chunk(tc):
        return xdst[tc % NPC].rearrange("(r k p) t -> r p k t", r=2, p=128)[tc // NPC]

    with ExitStack() as st:
        P = Prog(nc, st)
        for L in range(2):
            moe = (L % 2 == 1)
            last = (L == 1)
            P.begin_phase(f"a{L}")
            if L == 0:
                xc = lambda tc: xT_v[:, :, tc * TC:(tc + 1) * TC]
            else:
                xc = xd_chunk
            def y_piece(j, deps):
                P.collective("AllGather", [ysrc[j]], [ydst[j]], PAIRS, reads=deps, writes=[f"ydst{j}"], slot="agy")
            emit_p1(P, S, d, L, xc, ysrc_rows, y_piece, YP // TC)
            P.end_phase()
            P.begin_phase(f"c{L}")
            if L == 0:
                xch = lambda blk, c: xo_v[:, :, blk * TB + c * CH: blk * TB + (c + 1) * CH]
                och = xs_chunk
            else:
                xch = xs_chunk
                och = lambda blk, c: oT_v[:, :, blk * TB + c * CH: blk * TB + (c + 1) * CH]
            def x_piece(blk, c, deps):
                j = (blk * TB + c * CH) // XP
                P.collective("AllGather", [xsrc[j]], [xdst[j]], PAIRS, reads=deps, writes=[f"xdst{j}"], slot="agx")
            emit_p2(P, T, TB, moe, last, d, L, xch, ych, och, None if last else x_piece)
            P.end_phase()
    return nc


def fused_inputs(x, W, c):
    b, hs = c // 2, c % 2
    xTb = np.ascontiguousarray(x[b].T)
    p1 = [p1_inputs_T(None, L, hs, W) for L in range(2)]
    perm = np.concatenate([np.arange(0, 256), np.arange(512, 768), np.arange(256, 512), np.arange(768, 1024)])
    def pk(v):
        return np.ascontiguousarray(v.reshape(8, 128).T)
    vec2 = []
    for L in range(2):
        gy = np.concatenate([W["lru_out_norm"][L], W["sb_out_norm"][L]])[perm]
        vec2.append(np.stack([pk(gy), pk(W["ffn_norm"][L]), pk(W["final_norm"])], axis=1))
    hmask = np.zeros((128, 2), np.float32)
    hmask[:, hs] = 1.0
    return {
        "xT": xTb, "xT_own": np.ascontiguousarray(xTb[:, hs * T_CORE:(hs + 1) * T_CORE]),
        "gk1": np.stack([p["gk"] for p in p1]), "w_in": np.stack([p["w_in"] for p in p1]),
        "conv_w": np.stack([p["conv_w"] for p in p1]), "vecs1": np.stack([p["vecs"] for p in p1]),
        "w_r": np.stack([p["w_r"] for p in p1]), "w_i": np.stack([p["w_i"] for p in p1]),
        "consts1": p1_consts(),
        "vec2": np.ascontiguousarray(np.stack(vec2)), "w_out": np.ascontiguousarray(W["w_out"][:, perm, :]),
        "consts2": p2_consts(), "hmask": hmask,
        "dense_wg": W["dense_w_gate"], "dense_wu": W["dense_w_up"], "dense_wd": W["dense_w_down"],
        "moe_wg": W["moe_w_gate"][0], "moe_wu": W["moe_w_up"][0], "moe_wd": W["moe_w_down"][0],
        "w_router": W["router_w"][0],
    }


def kernel(**inputs):
    W = {k: np.asarray(v, dtype=np.float32) for k, v in inputs.items() if k != "x"}
    x = np.asarray(inputs["x"], dtype=np.float32)
    if "nc" not in _CACHE:
        _CACHE["nc"] = build_fused()
    cores = list(range(8))
    ins = [fused_inputs(x, W, c) for c in cores]
    res = run_bass_kernel_spmd(_CACHE["nc"], ins, core_ids=cores).results
    out = np.empty((4, S_FULL, 1024), np.float32)
    for c in cores:
        b, hs = c // 2, c % 2
        out[b, hs * T_CORE:(hs + 1) * T_CORE, :] = res[c]["outT"].T
    return out
```
